# Optimizing a Trainium2 kernel written in Bass

```python
import jax, jax.numpy as jnp
from jax import lax
import numpy as np


D_MODEL = 1024
BATCH = 16
SEQ = 4096
DEPTH = 4

CONV_WIDTH = D_MODEL
CONV_TAPS = 31
MLSTM_WIDTH = D_MODEL
MLSTM_HEADS = 4
MLSTM_HEAD_DIM = MLSTM_WIDTH // MLSTM_HEADS
QK_CONV_TAPS = 4
MLSTM_CHUNK = 64
N_EXPERTS = 32
TOP_K = 4
D_FF = D_MODEL
SWIGLU_LIMIT = 7.0
SWIGLU_ALPHA = 1.702
EXPERT_BLOCK = 128
LN_EPS = 1e-5
DEEPNORM_ALPHA = (2 * DEPTH) ** 0.25
DEEPNORM_BETA = (8 * DEPTH) ** -0.25
OFF_GLU = 0
OFF_Q = OFF_GLU + 2 * CONV_WIDTH
OFF_K = OFF_Q + MLSTM_WIDTH
OFF_V = OFF_K + MLSTM_WIDTH
OFF_O = OFF_V + MLSTM_WIDTH
OFF_I = OFF_O + MLSTM_WIDTH
OFF_F = OFF_I + MLSTM_HEADS
OFF_GA = OFF_F + MLSTM_HEADS
OFF_GB = OFF_GA + D_MODEL
N_IN = OFF_GB + D_MODEL

kernel_name = 'hybrid_conformer_mlstm_moe_deepnorm'


def layer_norm(x, g, b):
    xf = x.astype(jnp.float32)
    xc = xf - xf.mean(-1, keepdims=True)
    var = jnp.mean(xc * xc, -1, keepdims=True)
    y = xc * lax.rsqrt(var + LN_EPS) * g.astype(jnp.float32) + b.astype(jnp.float32)
    return y.astype(x.dtype)


def head_layer_norm(h, g):
    B, S, H, dh = h.shape
    hf = h.astype(jnp.float32)
    hc = hf - hf.mean(-1, keepdims=True)
    var = jnp.mean(hc * hc, -1, keepdims=True)
    return (hc * lax.rsqrt(var + LN_EPS)).reshape(B, S, H * dh) * g.astype(jnp.float32)


def causal_depthwise_conv(u, w, b):
    taps, C = w.shape
    y = lax.conv_general_dilated(u, w[:, None, :].astype(u.dtype), window_strides=(1,),
                                 padding=[(taps - 1, 0)], dimension_numbers=('NWC', 'WIO', 'NWC'),
                                 feature_group_count=C)
    return y + b.astype(u.dtype)


def conformer_branch(u, dw_w, dw_b, ng, nb, out_w, out_b):
    a, g = jnp.split(u, 2, axis=-1)
    z = a * jax.nn.sigmoid(g)
    z = causal_depthwise_conv(z, dw_w, dw_b)
    z = jax.nn.silu(layer_norm(z, ng, nb))
    return z @ out_w + out_b


def mlstm_chunkwise(q, k, v, i_pre, f_pre):
    B, S, H, dh = q.shape
    L = MLSTM_CHUNK
    nc = S // L
    f32 = jnp.float32

    def to_chunks(t):
        return t.astype(f32).reshape(B, nc, L, H, -1).transpose(1, 0, 3, 2, 4)

    def gate_chunks(t):
        return t.astype(f32).reshape(B, nc, L, H).transpose(1, 0, 3, 2)

    qc = to_chunks(q)
    kc = to_chunks(k) * (dh ** -0.5)
    vc = to_chunks(v)
    ic = gate_chunks(i_pre)
    bc = jnp.cumsum(gate_chunks(jax.nn.log_sigmoid(f_pre.astype(f32))), axis=-1)
    mask = jnp.tril(jnp.ones((L, L), dtype=bool))

    def step(carry, inp):
        C, n, m = carry
        q_, k_, v_, b, ig = inp
        dmat = jnp.where(mask, b[..., :, None] - b[..., None, :] + ig[..., None, :], -jnp.inf)
        inter = b + m[..., None]
        m_row = jnp.maximum(inter, dmat.max(-1))
        w_intra = jnp.exp(dmat - m_row[..., None])
        w_inter = jnp.exp(inter - m_row)
        s = jnp.einsum('bhtk,bhsk->bhts', q_, k_) * w_intra
        num = w_inter[..., None] * jnp.einsum('bhvk,bhtk->bhtv', C, q_) + jnp.einsum('bhts,bhsv->bhtv', s, v_)
        den = w_inter * jnp.einsum('bhk,bhtk->bht', n, q_) + s.sum(-1)
        h = num / jnp.maximum(jnp.abs(den), jnp.exp(-m_row))[..., None]
        b_last = b[..., -1]
        log_ws = b_last[..., None] - b + ig
        m_new = jnp.maximum(b_last + m, log_ws.max(-1))
        ws = jnp.exp(log_ws - m_new[..., None])
        decay = jnp.exp(b_last + m - m_new)
        C_new = decay[..., None, None] * C + jnp.einsum('bhsv,bhsk->bhvk', v_ * ws[..., None], k_)
        n_new = decay[..., None] * n + jnp.einsum('bhs,bhsk->bhk', ws, k_)
        return (C_new, n_new, m_new), h

    init = (jnp.zeros((B, H, dh, dh), f32), jnp.zeros((B, H, dh), f32), jnp.zeros((B, H), f32))
    _, h = lax.scan(step, init, (qc, kc, vc, bc, ic))
    return h.transpose(1, 0, 3, 2, 4).reshape(B, S, H, dh)


def mlstm_branch(q, k, v, o, i_pre, f_pre, qk_conv_w, qk_conv_b, norm_g, out_w):
    B, S, _ = q.shape
    qk = jax.nn.silu(causal_depthwise_conv(jnp.concatenate([q, k], -1), qk_conv_w, qk_conv_b))
    q, k = jnp.split(qk, 2, axis=-1)
    shp = (B, S, MLSTM_HEADS, MLSTM_HEAD_DIM)
    h = mlstm_chunkwise(q.reshape(shp), k.reshape(shp), v.reshape(shp), i_pre, f_pre)
    h = head_layer_norm(h, norm_g).astype(o.dtype) * jax.nn.sigmoid(o)
    return h @ out_w


def moe_ffn(x2d, router_w, router_b, w1, b1, w2, b2):
    T, D = x2d.shape
    logits = (x2d @ router_w + router_b).astype(jnp.float32)
    top_logit, top_idx = lax.top_k(logits, TOP_K)
    gate = jax.nn.softmax(top_logit, axis=-1)
    n_assign = T * TOP_K
    flat_e = top_idx.reshape(-1).astype(jnp.int32)
    flat_tok = jnp.arange(n_assign, dtype=jnp.int32) // TOP_K
    order = jnp.argsort(flat_e, stable=True)
    se, stok, sw = flat_e[order], flat_tok[order], gate.reshape(-1)[order]
    counts = jnp.zeros((N_EXPERTS,), jnp.int32).at[flat_e].add(1)
    starts = jnp.cumsum(counts) - counts
    padded = (counts + EXPERT_BLOCK - 1) // EXPERT_BLOCK * EXPERT_BLOCK
    pends = jnp.cumsum(padded)
    pstarts = pends - padded
    dest = pstarts[se] + (jnp.arange(n_assign, dtype=jnp.int32) - starts[se])
    n_blocks = -(-n_assign // EXPERT_BLOCK) + N_EXPERTS
    n_rows = n_blocks * EXPERT_BLOCK
    row_tok = jnp.full((n_rows,), T, jnp.int32).at[dest].set(stok)
    row_w = jnp.zeros((n_rows,), jnp.float32).at[dest].set(sw)
    block_e = jnp.minimum(jnp.searchsorted(pends, jnp.arange(n_blocks, dtype=jnp.int32) * EXPERT_BLOCK,
                                           side='right'), N_EXPERTS - 1)
    x_pad = jnp.concatenate([x2d, jnp.zeros((1, D), x2d.dtype)], axis=0)

    def expert_block(acc, blk):
        tok, w, e = blk
        h = x_pad[tok] @ w1[e] + b1[e]
        g, lin = jnp.split(h, 2, axis=-1)
        g = jnp.minimum(g, SWIGLU_LIMIT)
        lin = jnp.clip(lin, -SWIGLU_LIMIT, SWIGLU_LIMIT)
        act = g * jax.nn.sigmoid(SWIGLU_ALPHA * g) * (lin + 1.0)
        y = act @ w2[e] + b2[e]
        return acc.at[tok].add((w[:, None] * y).astype(acc.dtype)), None

    acc, _ = lax.scan(expert_block, jnp.zeros((T + 1, D), x2d.dtype),
                      (row_tok.reshape(n_blocks, EXPERT_BLOCK), row_w.reshape(n_blocks, EXPERT_BLOCK), block_e))
    return acc[:T]


def hybrid_layer(x, w_in, b_in, conv_dw_w, conv_dw_b, conv_norm_g, conv_norm_b, conv_out_w, conv_out_b,
                 qk_conv_w, qk_conv_b, mlstm_norm_g, mlstm_out_w, w_out, ln1_g, ln1_b,
                 router_w, router_b, moe_w1, moe_b1, moe_w2, moe_b2, ln2_g, ln2_b):
    B, S, D = x.shape
    xp = x @ w_in + b_in
    y_conv = conformer_branch(xp[..., OFF_GLU:OFF_Q], conv_dw_w, conv_dw_b, conv_norm_g, conv_norm_b,
                              conv_out_w, conv_out_b)
    y_mem = mlstm_branch(xp[..., OFF_Q:OFF_K], xp[..., OFF_K:OFF_V], xp[..., OFF_V:OFF_O], xp[..., OFF_O:OFF_I],
                         xp[..., OFF_I:OFF_F], xp[..., OFF_F:OFF_GA], qk_conv_w, qk_conv_b, mlstm_norm_g, mlstm_out_w)
    mix = jax.nn.sigmoid(xp[..., OFF_GA:OFF_GB]) * y_conv + jax.nn.sigmoid(xp[..., OFF_GB:N_IN]) * y_mem
    x = layer_norm(DEEPNORM_ALPHA * x + mix @ w_out, ln1_g, ln1_b)
    y = moe_ffn(x.reshape(B * S, D), router_w, router_b, moe_w1, moe_b1, moe_w2, moe_b2).reshape(B, S, D)
    return layer_norm(DEEPNORM_ALPHA * x + y, ln2_g, ln2_b)


def setup_inputs(seed: int = 0) -> dict:
    key = jax.random.key(seed)
    ks = jax.random.split(key, 24)
    f32 = jnp.float32
    L, D, H = DEPTH, D_MODEL, MLSTM_HEADS

    def nrm(k, shape, scale):
        return jax.random.normal(k, shape, f32) * scale

    col_scale = jnp.concatenate([
        jnp.full((OFF_V,), D ** -0.5, f32),
        jnp.full((MLSTM_WIDTH,), DEEPNORM_BETA * D ** -0.5, f32),
        jnp.full((MLSTM_WIDTH,), D ** -0.5, f32),
        jnp.full((2 * H,), 0.1 * D ** -0.5, f32),
        jnp.full((2 * D,), D ** -0.5, f32)])
    w_in = nrm(ks[1], (L, D, N_IN), 1.0) * col_scale
    b_in = jnp.concatenate([
        nrm(ks[2], (L, OFF_I), 0.02),
        nrm(ks[3], (L, H), 0.1),
        jnp.linspace(3.0, 6.0, H, dtype=f32) + nrm(ks[4], (L, H), 0.1),
        nrm(ks[5], (L, 2 * D), 0.02)], axis=-1)
    return {
        'x': nrm(ks[0], (BATCH, SEQ, D), 1.0),
        'w_in': w_in,
        'b_in': b_in,
        'conv_dw_w': nrm(ks[6], (L, CONV_TAPS, CONV_WIDTH), CONV_TAPS ** -0.5),
        'conv_dw_b': nrm(ks[7], (L, CONV_WIDTH), 0.02),
        'conv_norm_g': 1.0 + nrm(ks[8], (L, CONV_WIDTH), 0.02),
        'conv_norm_b': nrm(ks[9], (L, CONV_WIDTH), 0.02),
        'conv_out_w': nrm(ks[10], (L, CONV_WIDTH, D), DEEPNORM_BETA * CONV_WIDTH ** -0.5),
        'conv_out_b': nrm(ks[11], (L, D), 0.02),
        'qk_conv_w': nrm(ks[12], (L, QK_CONV_TAPS, 2 * MLSTM_WIDTH), QK_CONV_TAPS ** -0.5),
        'qk_conv_b': nrm(ks[13], (L, 2 * MLSTM_WIDTH), 0.02),
        'mlstm_norm_g': 1.0 + nrm(ks[14], (L, MLSTM_WIDTH), 0.02),
        'mlstm_out_w': nrm(ks[15], (L, MLSTM_WIDTH, D), DEEPNORM_BETA * MLSTM_WIDTH ** -0.5),
        'w_out': nrm(ks[16], (L, D, D), DEEPNORM_BETA * D ** -0.5),
        'ln1_g': 1.0 + nrm(ks[17], (L, D), 0.02),
        'ln1_b': nrm(ks[18], (L, D), 0.02),
        'router_w': nrm(ks[19], (L, D, N_EXPERTS), D ** -0.5),
        'router_b': nrm(ks[20], (L, N_EXPERTS), 0.01),
        'moe_w1': nrm(ks[21], (L, N_EXPERTS, D, 2 * D_FF), D ** -0.5),
        'moe_b1': nrm(ks[22], (L, N_EXPERTS, 2 * D_FF), 0.02),
        'moe_w2': nrm(ks[23], (L, N_EXPERTS, D_FF, D), DEEPNORM_BETA * D_FF ** -0.5),
        'moe_b2': nrm(jax.random.fold_in(ks[23], 1), (L, N_EXPERTS, D), 0.02),
        'ln2_g': 1.0 + nrm(jax.random.fold_in(ks[17], 1), (L, D), 0.02),
        'ln2_b': nrm(jax.random.fold_in(ks[18], 1), (L, D), 0.02),
    }


def reference(x, w_in, b_in, conv_dw_w, conv_dw_b, conv_norm_g, conv_norm_b, conv_out_w, conv_out_b,
              qk_conv_w, qk_conv_b, mlstm_norm_g, mlstm_out_w, w_out, ln1_g, ln1_b,
              router_w, router_b, moe_w1, moe_b1, moe_w2, moe_b2, ln2_g, ln2_b):
    for l in range(DEPTH):
        x = hybrid_layer(x, w_in[l], b_in[l], conv_dw_w[l], conv_dw_b[l], conv_norm_g[l], conv_norm_b[l],
                         conv_out_w[l], conv_out_b[l], qk_conv_w[l], qk_conv_b[l], mlstm_norm_g[l],
                         mlstm_out_w[l], w_out[l], ln1_g[l], ln1_b[l], router_w[l], router_b[l],
                         moe_w1[l], moe_b1[l], moe_w2[l], moe_b2[l], ln2_g[l], ln2_b[l])
    return x
```

```python
import math
from contextlib import ExitStack

import numpy as np
import concourse.bass as bass
import concourse.mybir as mybir
from concourse.bass_utils import run_bass_kernel_spmd

F32 = mybir.dt.float32; BF16 = mybir.dt.bfloat16; U32 = mybir.dt.uint32; I32 = mybir.dt.int32
AF = mybir.ActivationFunctionType; ALU = mybir.AluOpType

NCORES = 8
D = 1024; KC = 8; P = 128
NT = 8192; SEQ = 4096; TB = 512; NBLK = NT // TB; BPS = SEQ // TB; NTILE = NT // P
DEPTH = 4
NE = 32; TOPK = 4; DFF = 1024
CAP = 1280; NSLOT = NE * CAP; EBLK = [(0, 512), (512, 512), (1024, 256)]
TAPS = 31
OFF_Q = 2048; OFF_K = 3072; OFF_V = 4096; OFF_O = 5120; OFF_I = 6144; OFF_GA = 6152; OFF_GB = 7176; N_IN = 8200
ALPHA = (2 * DEPTH) ** 0.25
EPS = 1e-5
LIMIT = 7.0; SW_ALPHA = 1.702

PCOL = {}
_o = 0
for _n, _w in [("b_a", 8), ("b_g", 8), ("b_q", 8), ("b_k", 8), ("b_o", 8), ("b_ga", 8), ("b_gb", 8),
               ("dw_w", 8 * TAPS), ("dw_b", 8), ("cn_g", 8), ("cn_b", 8), ("co_b", 8),
               ("qk_w", 16 * 4), ("qk_b", 16), ("mn_g", 8), ("b1", NE * 16)]:
    PCOL[_n] = _o; _o += _w
NCOL = _o
PROW = {"b_v": 0, "b_if": 1024, "r_b": 1032, "b2": 1064}
NROW = 1064 + NE * D


class Buf:
    __slots__ = ("name", "w", "r")

    def __init__(self, name):
        self.name = name; self.w = None; self.r = []


class FW:
    def __init__(self, nc):
        self.nc = nc
        self.engs = {"pe": nc.tensor, "act": nc.scalar, "dve": nc.vector, "pool": nc.gpsimd, "sp": nc.sync}
        self.sem = {k: nc.alloc_semaphore(name=f"sem_{k}") for k in ("pe", "act", "dve", "pool")}
        self.cnt = {k: 0 for k in self.sem}
        self.seen = {k: {} for k in self.engs}
        self.dsem = {}
        self.nwait = 0; self.nins = 0

    def _semh(self, key):
        return self.sem[key] if key in self.sem else self.dsem[key][0]

    def _wait(self, eng, k, v):
        seen = self.seen[eng]
        if v <= 0 or seen.get(k, 0) >= v:
            return
        self.engs[eng].wait_ge(self._semh(k), v)
        seen[k] = v; self.nwait += 1

    def _sync(self, eng, reads, writes, extra=()):
        need = {}
        def add(ev):
            k, v = ev
            if k == eng and eng == "pe":
                return
            if v > need.get(k, 0): need[k] = v
        for ev in extra: add(ev)
        for b in reads:
            if b.w is not None: add(b.w)
        for b in writes:
            if b.w is not None and b.w[0] != eng: add(b.w)
            for ev in b.r: add(ev)
        for k, v in need.items():
            self._wait(eng, k, v)

    def _record(self, ev, reads, writes):
        for b in reads:
            b.r.append(ev)
            if len(b.r) > 24:
                m = {}
                for k, v in b.r:
                    if v > m.get(k, 0): m[k] = v
                b.r = list(m.items())
        for b in writes:
            b.w = ev; b.r = []

    def op(self, eng, fn, reads=(), writes=()):
        self._sync(eng, reads, writes)
        ins = fn(self.engs[eng])
        self.cnt[eng] += 1; self.nins += 1
        ins.then_inc(self.sem[eng], 1)
        self._record((eng, self.cnt[eng]), reads, writes)
        return ins

    def dma(self, q, key, fn, reads=(), writes=(), inc=16):
        if key not in self.dsem:
            self.dsem[key] = [self.nc.alloc_semaphore(name=f"d_{key}"), 0]
        s = self.dsem[key]
        self._sync(q, reads, writes, [(key, s[1])])
        ins = fn(self.engs[q])
        s[1] += inc; self.nins += 1
        ins.then_inc(s[0], inc)
        self._record((key, s[1]), reads, writes)
        return ins

    def barrier(self, engs=("pe", "act", "dve", "pool", "sp")):
        for e in engs:
            for k in self.sem:
                if k != e: self._wait(e, k, self.cnt[k])
            for k, (s, c) in self.dsem.items():
                if k != "cc" and not k.startswith("gcp"):
                    self._wait(e, k, c)


class Builder:
    def __init__(self, Lp, debug=False):
        self.Lp = Lp; self.debug = debug
        nc = self.nc = bass.Bass("TRN2", target_bir_lowering=False)
        self.fw = FW(nc)
        self.dbufs = {}
        dt = lambda name, shape, dtype, kind: nc.dram_tensor(name, shape, dtype, kind=kind).ap()
        EI = "ExternalInput"
        self.x_in = dt("x", [NT, D], F32, EI)
        self.out = dt("out", [NT, D], F32, "ExternalOutput")
        self.w_in_sh = dt("w_in_sh", [Lp, P, N_IN], F32, EI)
        self.co_sh = dt("co_sh", [Lp, P, D], F32, EI)
        self.mo_sh = dt("mo_sh", [Lp, P, D], F32, EI)
        self.wo_sh = dt("wo_sh", [Lp, P, D], F32, EI)
        self.w1_sh = dt("w1_sh", [Lp, 4 * D, 2 * DFF], F32, EI)
        self.w2_sh = dt("w2_sh", [Lp, 4 * DFF, D], F32, EI)
        self.pcol = dt("pcol", [Lp, P, NCOL], F32, EI)
        self.prow = dt("prow", [Lp, 1, NROW], F32, EI)
        self.pbc = dt("pbc", [Lp, 4, D], F32, EI)
        self.rw = dt("rw", [Lp, D, NE], F32, EI)
        IN = "Internal"
        dk = "ExternalOutput" if debug else IN
        self.s_win = [dt(f"s_win{l}", [P, N_IN], F32, IN) for l in range(Lp)]
        self.g_win = [dt(f"g_win{l}", [D, N_IN], F32, IN) for l in range(Lp)]
        self.s_sm = [[dt(f"s_sm{l}_{i}", [P, D], F32, IN) for i in range(3)] for l in range(Lp)]
        self.g_sm = [[dt(f"g_sm{l}_{i}", [D, D], F32, IN) for i in range(3)] for l in range(Lp)]
        self.s_w1 = [dt(f"s_w1{l}", [4 * D, 2 * DFF], F32, IN) for l in range(Lp)]
        self.g_w1 = [dt(f"g_w1{l}", [NE * D, 2 * DFF], F32, IN) for l in range(Lp)]
        self.s_w2 = [dt(f"s_w2{l}", [4 * DFF, D], F32, IN) for l in range(Lp)]
        self.g_w2 = [dt(f"g_w2{l}", [NE * DFF, D], F32, IN) for l in range(Lp)]
        self.XT = dt("XT", [NBLK, P, KC, TB], BF16, IN)
        self.GCT = dt("GCT", [NBLK, P, KC, TB], BF16, dk)
        self.HGT = dt("HGT", [NBLK, P, KC, TB], BF16, dk)
        self.X1 = dt("X1", [NT, D], F32, dk)
        self.XS = dt("XS", [NSLOT, D], BF16, IN)
        self.YS = dt("YS", [NSLOT + 1, D], BF16, IN)
        self.XN = [dt(f"XN{i}", [NT, D], F32, IN) for i in range(2)] if Lp > 1 else []
        if debug:
            self.dbg_dest = dt("dbg_dest", [P, NTILE * 4], I32, "ExternalOutput")
            self.dbg_gate = dt("dbg_gate", [P, NTILE * 4], F32, "ExternalOutput")
            self.dbg_cnt = dt("dbg_cnt", [1, NE], F32, "ExternalOutput")

    def DB(self, name):
        b = self.dbufs.get(name)
        if b is None:
            b = self.dbufs[name] = Buf(name)
        return b

    def sb(self, st, name, shape, dtype):
        self.uid = getattr(self, "uid", 0) + 1
        t = st.enter_context(self.nc.sbuf_tensor(f"{name}_u{self.uid}", shape, dtype))
        return t.ap() if hasattr(t, "ap") and callable(getattr(t, "ap")) else t

    def banks(self, st):
        res = []
        for i in range(8):
            self.uid = getattr(self, "uid", 0) + 1
            t = st.enter_context(self.nc.psum_tensor(f"bank{i}_u{self.uid}", [P, 512], F32))
            res.append((t.ap() if callable(getattr(t, "ap", None)) else t, Buf(f"bank{i}")))
        return res

    def consts(self, st):
        fw = self.fw; sb = self.sb
        c = self.c = {}
        c["b"] = Buf("consts")
        cb = c["b"]
        c["ident_bf"] = sb(st, "ident_bf", [P, P], BF16)
        c["ident_f"] = sb(st, "ident_f", [P, P], F32)
        c["ones_bf"] = sb(st, "ones_bf", [P, P], BF16)
        c["ones_f"] = sb(st, "ones_f", [P, P], F32)
        c["tri_incl"] = sb(st, "tri_incl", [P, P], F32)
        c["tri_strict"] = sb(st, "tri_strict", [P, P], F32)
        c["iota_e"] = sb(st, "iota_e", [P, NE], F32)
        c["ecap"] = sb(st, "ecap", [P, NE], F32)
        iota_i = sb(st, "iota_i", [P, NE], I32)
        c["zrow"] = sb(st, "zrow", [1, D], BF16)
        c["zt"] = sb(st, "zt", [P, 8, D], BF16)
        g = "pool"
        for nm, val in [("ident_bf", 1.0), ("ident_f", 1.0), ("ones_bf", 1.0), ("ones_f", 1.0), ("tri_incl", 1.0), ("tri_strict", 1.0), ("zrow", 0.0), ("zt", 0.0)]:
            fw.op(g, lambda e, nm=nm, val=val: e.memset(c[nm], val), writes=[cb])
        for nm, op_, base in [("ident_bf", ALU.is_equal, 0), ("ident_f", ALU.is_equal, 0), ("tri_incl", ALU.is_ge, 0), ("tri_strict", ALU.is_gt, 0)]:
            fw.op(g, lambda e, nm=nm, op_=op_, base=base: e.affine_select(out=c[nm], in_=c[nm], pattern=[[1, P]], compare_op=op_, fill=0.0, base=base, channel_multiplier=-1),
                  reads=[cb], writes=[cb])
        fw.op(g, lambda e: e.iota(iota_i, pattern=[[1, NE]], base=0, channel_multiplier=0), writes=[cb])
        fw.op("dve", lambda e: e.tensor_copy(out=c["iota_e"], in_=iota_i), reads=[cb], writes=[cb])
        fw.op("dve", lambda e: e.tensor_scalar(out=c["ecap"], in0=c["iota_e"], scalar1=float(CAP), scalar2=None, op0=ALU.mult), reads=[cb], writes=[cb])
        fw.dma("sp", "zrow", lambda e: e.dma_start(out=self.YS[NSLOT:NSLOT + 1, :], in_=c["zrow"]), reads=[cb])
        c["dest"] = sb(st, "dest", [P, NTILE * 4], I32)
        c["gate"] = sb(st, "gate", [P, NTILE * 4], F32)
        c["bdest"] = Buf("dest")
        c["r_sc"] = self.nc.gpsimd.to_reg(NSLOT - 1)
        c["r_ga"] = self.nc.gpsimd.to_reg(NSLOT)

    def gathers(self, l, parts=("win", "sm", "w1", "w2")):
        fw = self.fw
        rg = [list(range(NCORES))]
        def one(name, shard_ap, s_ap, g_ap, rows):
            CH = 1024
            for r0 in range(0, rows, CH):
                r1 = min(rows, r0 + CH)
                self.ngcp = getattr(self, "ngcp", 0) + 1
                fw.dma("sp", f"gcp{self.ngcp % 4}", lambda e: e.dma_start(out=s_ap[r0:r1, :], in_=shard_ap[r0:r1, :]), writes=[self.DB("s_" + name)])
            fw.dma("pool", "cc", lambda e: e.collective_compute("AllGather", ALU.bypass, replica_groups=rg, ins=[s_ap], outs=[g_ap]),
                   reads=[self.DB("s_" + name)], writes=[self.DB("g_" + name)], inc=1)
        if "win" in parts:
            one(f"win{l}", self.w_in_sh[l], self.s_win[l], self.g_win[l], P)
        if "sm" in parts:
            for i, sh in enumerate([self.co_sh, self.mo_sh, self.wo_sh]):
                one(f"sm{l}_{i}", sh[l], self.s_sm[l][i], self.g_sm[l][i], P)
        if "w1" in parts:
            one(f"w1{l}", self.w1_sh[l], self.s_w1[l], self.g_w1[l], 4 * D)
        if "w2" in parts:
            one(f"w2{l}", self.w2_sh[l], self.s_w2[l], self.g_w2[l], 4 * DFF)

    def load_w(self, dst, src2d, c0, c1, rbuf, wbuf, key):
        srcr = src2d.rearrange("(kc p) n -> p kc n", p=P)
        o = 0
        while c0 + o < c1:
            w = min(2048, c1 - c0 - o)
            self.fw.dma("pool", key, lambda e, o=o, w=w: e.dma_start(out=dst[:, :, o:o + w], in_=srcr[:, :, c0 + o:c0 + o + w]),
                        reads=[rbuf], writes=[wbuf])
            o += w

    def ln_tok(self, r, out, G, Bt, st6, mv, sd, rs, bufs_r, bufs_w, cbuf):
        fw = self.fw
        for hlf in range(2):
            fw.op("dve", lambda e, hlf=hlf: e.bn_stats(out=st6[:, hlf, :], in_=r[:, hlf * 512:(hlf + 1) * 512]), reads=bufs_r, writes=[bufs_w[0]])
        fw.op("dve", lambda e: e.bn_aggr(out=mv, in_=st6), reads=[bufs_w[0]], writes=[bufs_w[0]])
        fw.op("act", lambda e: e.activation(out=sd, in_=mv[:, 1:2], func=AF.Sqrt, bias=self.c["eps"], scale=1.0), reads=[bufs_w[0]], writes=[bufs_w[0]])
        fw.op("dve", lambda e: e.reciprocal(out=rs, in_=sd), reads=[bufs_w[0]], writes=[bufs_w[0]])
        fw.op("dve", lambda e: e.tensor_scalar(out=r, in0=r, scalar1=mv[:, 0:1], scalar2=rs, op0=ALU.subtract, op1=ALU.mult), reads=bufs_r + [bufs_w[0]], writes=bufs_r)
        fw.op("dve", lambda e: e.tensor_tensor(out=r, in0=r, in1=G, op=ALU.mult), reads=bufs_r + [cbuf], writes=bufs_r)
        fw.op("dve", lambda e: e.tensor_tensor(out=out, in0=r, in1=Bt, op=ALU.add), reads=bufs_r + [cbuf], writes=[bufs_w[1]])

    def pass_x0(self, x_src, xkey):
        fw = self.fw; c = self.c
        with ExitStack() as st:
            bk = self.banks(st)
            xt = [self.sb(st, f"x0_x{i}", [P, D], F32) for i in range(2)]
            xb = [self.sb(st, f"x0_xb{i}", [P, D], BF16) for i in range(2)]
            xT = [self.sb(st, f"x0_T{i}", [P, KC, TB], BF16) for i in range(2)]
            bx = [Buf("x0x0"), Buf("x0x1")]; bxb = [Buf("x0b0"), Buf("x0b1")]; bT = [Buf("x0T0"), Buf("x0T1")]
            def ld(t):
                fw.dma("sp", f"x0ld{t % 2}", lambda e: e.dma_start(out=xt[t % 2], in_=x_src[t * P:(t + 1) * P, :]), reads=[self.DB(xkey)], writes=[bx[t % 2]])
            ld(0)
            for t in range(NTILE):
                i = t % 2; blk = t // 4; j = t % 4
                if t + 1 < NTILE: ld(t + 1)
                fw.op("act", lambda e: e.activation(out=xb[i], in_=xt[i], func=AF.Copy), reads=[bx[i]], writes=[bxb[i]])
                pb, pbuf = bk[t % 2]
                pv = pb.bitcast(BF16)
                for kc in range(KC):
                    fw.op("pe", lambda e, kc=kc: e.transpose(out=pv[:, kc * P:(kc + 1) * P], in_=xb[i][:, kc * P:(kc + 1) * P], identity=c["ident_bf"]),
                          reads=[bxb[i], c["b"]], writes=[pbuf])
                T = xT[blk % 2]
                fw.op("dve", lambda e: e.tensor_copy(out=T[:, :, j * P:(j + 1) * P], in_=pv.rearrange("p (k t) -> p k t", k=KC)), reads=[pbuf], writes=[bT[blk % 2]])
                if j == 3:
                    fw.dma("sp", f"x0st{blk % 2}", lambda e: e.dma_start(out=self.XT[blk], in_=T), reads=[bT[blk % 2]], writes=[self.DB("XT")])
            fw.barrier()

    def zero_scratch(self):
        fw = self.fw; c = self.c
        zt = c["zt"]
        R = 1024
        i = 0
        for tgt, rows in ((self.XS, NSLOT), (self.YS, NSLOT)):
            for r0 in range(0, rows, R):
                fw.dma("sp", f"zf{i % 4}", lambda e: e.dma_start(out=tgt[r0:r0 + R, :].rearrange("(p j) d -> p j d", p=P), in_=zt), reads=[c["b"]])
                i += 1

    def pass_c(self, l):
        fw = self.fw; c = self.c; cb = c["b"]
        with ExitStack() as st:
            sb = lambda n, s, d: self.sb(st, n, s, d)
            bk = self.banks(st)
            Wc = sb("c_W", [P, KC, 3072], BF16); Wco = sb("c_Wco", [P, KC, D], BF16)
            bW = Buf("c_W")
            pc = c["pcol"]; bpc = c["bpcol"]
            self.load_w(Wc[:, :, 0:2048], self.g_win[l], 0, 2048, self.DB(f"g_win{l}"), bW, "wl0")
            self.load_w(Wc[:, :, 2048:3072], self.g_win[l], OFF_GA, OFF_GA + 1024, self.DB(f"g_win{l}"), bW, "wl1")
            self.load_w(Wco, self.g_sm[l][0], 0, D, self.DB(f"g_sm{l}_0"), bW, "wl2")
            if l + 1 < self.Lp:
                self.gathers(l + 1)
            xT = [sb(f"c_xT{i}", [P, KC, TB], BF16) for i in range(2)]; bxT = [Buf("c_xT0"), Buf("c_xT1")]
            z = sb("c_z", [P, KC, TB + 30], BF16); bz = [Buf(f"c_z{i}") for i in range(KC)]
            dg = [sb(f"c_dg{i}", [P, TAPS, P], BF16) for i in range(2)]; bdg = [Buf("c_dg0"), Buf("c_dg1")]
            cv = sb("c_cv", [P, KC, TB], F32); bcv = [Buf(f"c_cv{i}") for i in range(KC)]
            sg = [sb(f"c_sg{i}", [P, TB], F32) for i in range(2)]; bsg = [Buf("c_sg0"), Buf("c_sg1")]
            zb = [sb(f"c_zb{i}", [P, TB], BF16) for i in range(2)]; bzb = [Buf("c_zb0"), Buf("c_zb1")]
            zs = [sb(f"c_zs{i}", [P, TB], BF16) for i in range(2)]; bzs = [Buf("c_zs0"), Buf("c_zs1")]
            mean = sb("c_mean", [P, TB], F32); m2 = sb("c_m2", [P, TB], F32); rstd = sb("c_rstd", [P, TB], F32); bst = Buf("c_stats")
            zn = sb("c_zn", [P, KC, TB], BF16); bzn = Buf("c_zn")
            gct = [sb(f"c_gct{i}", [P, KC, TB], BF16) for i in range(2)]; bgct = [Buf("c_gct0"), Buf("c_gct1")]
            col = lambda nm, i: pc[:, PCOL[nm] + i:PCOL[nm] + i + 1]
            S1, bS1 = bk[4]; S2, bS2 = bk[5]
            def ld(b):
                fw.dma("sp", f"c_ld{b % 2}", lambda e: e.dma_start(out=xT[b % 2], in_=self.XT[b]), reads=[self.DB("XT")], writes=[bxT[b % 2]])
            ld(0)
            for b in range(NBLK):
                i = b % 2
                if b + 1 < NBLK: ld(b + 1)
                if b % BPS == 0:
                    fw.op("dve", lambda e: e.memset(z[:, :, 0:30], 0.0), writes=bz)
                for ch in range(KC):
                    r = ch % 2
                    pa, bpa = bk[2 * r]; pg, bpg = bk[2 * r + 1]
                    for kc in range(KC):
                        fw.op("pe", lambda e, kc=kc: e.matmul(pa, lhsT=Wc[:, kc, ch * P:(ch + 1) * P], rhs=xT[i][:, kc, :], start=(kc == 0), stop=(kc == KC - 1)),
                              reads=[bW, bxT[i]], writes=[bpa])
                    for kc in range(KC):
                        fw.op("pe", lambda e, kc=kc: e.matmul(pg, lhsT=Wc[:, kc, D + ch * P:D + (ch + 1) * P], rhs=xT[i][:, kc, :], start=(kc == 0), stop=(kc == KC - 1)),
                              reads=[bW, bxT[i]], writes=[bpg])
                    fw.op("act", lambda e: e.activation(out=sg[r], in_=pg, func=AF.Sigmoid, bias=col("b_g", ch), scale=1.0), reads=[bpg, bpc], writes=[bsg[r]])
                    fw.op("dve", lambda e: e.scalar_tensor_tensor(out=z[:, ch, 30:30 + TB], in0=pa, scalar=col("b_a", ch), in1=sg[r], op0=ALU.add, op1=ALU.mult),
                          reads=[bpa, bsg[r], bpc], writes=[bz[ch]])
                    wcol = lambda j: pc[:, PCOL["dw_w"] + ch * TAPS + j:PCOL["dw_w"] + ch * TAPS + j + 1]
                    for j in range(TAPS):
                        fw.op("dve", lambda e, j=j: e.tensor_scalar(out=dg[r][:, j, :], in0=c["ident_bf"], scalar1=wcol(j), scalar2=None, op0=ALU.mult), reads=[cb, bpc], writes=[bdg[r]])
                    pcv, bpcv = bk[6 + r]
                    for j in range(TAPS):
                        fw.op("pe", lambda e, j=j: e.matmul(pcv, lhsT=dg[r][:, j, :], rhs=z[:, ch, j:j + TB], start=(j == 0), stop=(j == TAPS - 1)), reads=[bdg[r], bz[ch]], writes=[bpcv])
                    fw.op("act", lambda e: e.activation(out=z[:, ch, 0:30], in_=z[:, ch, TB:TB + 30], func=AF.Copy), reads=[bz[ch]], writes=[bz[ch]])
                    fw.op("act", lambda e: e.activation(out=cv[:, ch, :], in_=pcv, func=AF.Identity, bias=col("dw_b", ch), scale=1.0), reads=[bpcv, bpc], writes=[bcv[ch]])
                    fw.op("act", lambda e: e.activation(out=zb[r], in_=pcv, func=AF.Identity, bias=col("dw_b", ch), scale=1.0), reads=[bpcv, bpc], writes=[bzb[r]])
                    fw.op("act", lambda e: e.activation(out=zs[r], in_=pcv, func=AF.Square, bias=col("dw_b", ch), scale=1.0), reads=[bpcv, bpc], writes=[bzs[r]])
                    fw.op("pe", lambda e: e.matmul(S1, lhsT=c["ones_bf"], rhs=zb[r], start=(ch == 0), stop=(ch == KC - 1)), reads=[cb, bzb[r]], writes=[bS1])
                    fw.op("pe", lambda e: e.matmul(S2, lhsT=c["ones_bf"], rhs=zs[r], start=(ch == 0), stop=(ch == KC - 1)), reads=[cb, bzs[r]], writes=[bS2])
                fw.op("act", lambda e: e.activation(out=mean, in_=S1, func=AF.Identity, scale=1.0 / D), reads=[bS1], writes=[bst])
                fw.op("dve", lambda e: e.tensor_tensor(out=m2, in0=mean, in1=mean, op=ALU.mult), reads=[bst], writes=[bst])
                fw.op("dve", lambda e: e.scalar_tensor_tensor(out=m2, in0=S2, scalar=1.0 / D, in1=m2, op0=ALU.mult, op1=ALU.subtract), reads=[bS2, bst], writes=[bst])
                fw.op("act", lambda e: e.activation(out=m2, in_=m2, func=AF.Sqrt, bias=c["eps"], scale=1.0), reads=[bst], writes=[bst])
                fw.op("dve", lambda e: e.reciprocal(out=rstd, in_=m2), reads=[bst], writes=[bst])
                for ch in range(KC):
                    fw.op("dve", lambda e: e.tensor_tensor(out=cv[:, ch, :], in0=cv[:, ch, :], in1=mean, op=ALU.subtract), reads=[bcv[ch], bst], writes=[bcv[ch]])
                    fw.op("dve", lambda e: e.tensor_tensor(out=cv[:, ch, :], in0=cv[:, ch, :], in1=rstd, op=ALU.mult), reads=[bcv[ch], bst], writes=[bcv[ch]])
                    fw.op("act", lambda e: e.activation(out=zn[:, ch, :], in_=cv[:, ch, :], func=AF.Silu, bias=col("cn_b", ch), scale=col("cn_g", ch)),
                          reads=[bcv[ch], bpc], writes=[bzn])
                for d in range(KC):
                    r = d % 2
                    py, bpy = bk[2 * r]; pga, bpga = bk[2 * r + 1]
                    for ch in range(KC):
                        fw.op("pe", lambda e, ch=ch: e.matmul(py, lhsT=Wco[:, ch, d * P:(d + 1) * P], rhs=zn[:, ch, :], start=(ch == 0), stop=(ch == KC - 1)),
                              reads=[bW, bzn], writes=[bpy])
                    for kc in range(KC):
                        fw.op("pe", lambda e, kc=kc: e.matmul(pga, lhsT=Wc[:, kc, 2048 + d * P:2048 + (d + 1) * P], rhs=xT[i][:, kc, :], start=(kc == 0), stop=(kc == KC - 1)),
                              reads=[bW, bxT[i]], writes=[bpga])
                    fw.op("act", lambda e: e.activation(out=sg[r], in_=pga, func=AF.Sigmoid, bias=col("b_ga", d), scale=1.0), reads=[bpga, bpc], writes=[bsg[r]])
                    fw.op("dve", lambda e: e.scalar_tensor_tensor(out=gct[i][:, d, :], in0=py, scalar=col("co_b", d), in1=sg[r], op0=ALU.add, op1=ALU.mult),
                          reads=[bpy, bsg[r], bpc], writes=[bgct[i]])
                fw.dma("sp", f"c_st{i}", lambda e: e.dma_start(out=self.GCT[b], in_=gct[i]), reads=[bgct[i]], writes=[self.DB("GCT")])
            fw.barrier()

    def pass_m1(self, l):
        fw = self.fw; c = self.c; cb = c["b"]
        with ExitStack() as st:
            sb = lambda n, s, d: self.sb(st, n, s, d)
            bk = self.banks(st)
            NW = 4104
            Wm = sb("m_W", [P, KC, NW], BF16); bW = Buf("m_W")
            self.load_w(Wm, self.g_win[l], OFF_Q, OFF_Q + NW, self.DB(f"g_win{l}"), bW, "wl0")
            pc = c["pcol"]; bpc = c["bpcol"]; pr = c["prow_bf"]; bpr = c["bprow"]
            col = lambda nm, i: pc[:, PCOL[nm] + i:PCOL[nm] + i + 1]
            xT = [sb(f"m_xT{i}", [P, KC, TB], BF16) for i in range(2)]; bxT = [Buf("m_xT0"), Buf("m_xT1")]
            raw = [sb(f"m_raw{i}", [P, TB + 3], F32) for i in range(2)]; braw = [Buf("m_raw0"), Buf("m_raw1")]
            halo = sb("m_halo", [P, 16, 3], F32); bhalo = Buf("m_halo")
            acc = [sb(f"m_acc{i}", [P, TB], F32) for i in range(2)]; bacc = [Buf("m_acc0"), Buf("m_acc1")]
            qkT = sb("m_qkT", [P, 16, TB], BF16); bqk = Buf("m_qkT")
            soT = sb("m_soT", [P, KC, TB], BF16); bso = Buf("m_soT")
            hgt = [sb(f"m_hgt{i}", [P, KC, TB], BF16) for i in range(2)]; bhgt = [Buf("m_hgt0"), Buf("m_hgt1")]
            vp = [sb(f"m_vp{i}", [P, 4, 257], BF16) for i in range(2)]; bvp = [Buf("m_vp0"), Buf("m_vp1")]
            C = sb("m_C", [P, 4, 2, 257], F32); Cb = sb("m_Cb", [P, 4, 2, 257], BF16); bC = [Buf(f"m_C{h}") for h in range(4)]
            E = sb("m_E", [P, 512], F32); bE = Buf("m_E")
            lfb = sb("m_lfb", [P, 4, P], F32); blfb = Buf("m_lfb")
            ifs = sb("m_ifs", [P, 8], F32); ef = sb("m_ef", [P, 4], F32); lfn = sb("m_lfn", [P, 4], F32)
            arg = sb("m_arg", [P, 4], F32); wk = sb("m_wk", [P, 4], F32); bg = Buf("m_gates")
            qtl = [sb(f"m_qtl{h}", [P, 2, P], BF16) for h in range(4)]; bqtl = [Buf(f"m_qtl{h}") for h in range(4)]
            AT = [sb(f"m_AT{h}", [P, P], BF16) for h in range(4)]; bAT = [Buf(f"m_AT{h}") for h in range(4)]
            ktok = [sb(f"m_ktok{h}", [P, 256], BF16) for h in range(4)]; bktok = [Buf(f"m_ktok{h}") for h in range(4)]
            hh = [sb(f"m_hh{h}", [P, 256], F32) for h in range(4)]; bhh = [Buf(f"m_hh{h}") for h in range(4)]
            hn = [sb(f"m_hn{h}", [P, 256], BF16) for h in range(4)]; bhn = [Buf(f"m_hn{h}") for h in range(4)]
            sm = [sb(f"m_sm{h}", [P, 16], F32) for h in range(4)]; bsm = [Buf(f"m_sm{h}") for h in range(4)]
            stt = [sb(f"m_stt{i}", [P, 257], F32) for i in range(4)]; bstt = [Buf(f"m_stt{i}") for i in range(4)]
            for i in range(2):
                fw.op("dve", lambda e, i=i: e.memset(vp[i][:, :, 256:257], 1.0), writes=[bvp[i]])
            def ld(b):
                fw.dma("sp", f"m_ld{b % 2}", lambda e: e.dma_start(out=xT[b % 2], in_=self.XT[b]), reads=[self.DB("XT")], writes=[bxT[b % 2]])
            ld(0)
            for b in range(NBLK):
                i = b % 2
                if b + 1 < NBLK: ld(b + 1)
                if b % BPS == 0:
                    fw.op("dve", lambda e: e.memset(halo, 0.0), writes=[bhalo])
                    fw.op("dve", lambda e: e.memset(C, 0.0), writes=bC)
                    fw.op("dve", lambda e: e.memset(Cb, 0.0), writes=bC)
                for ch in range(16):
                    r = ch % 2
                    pq, bpq = bk[r]
                    for kc in range(KC):
                        fw.op("pe", lambda e, kc=kc: e.matmul(pq, lhsT=Wm[:, kc, ch * P:(ch + 1) * P], rhs=xT[i][:, kc, :], start=(kc == 0), stop=(kc == KC - 1)),
                              reads=[bW, bxT[i]], writes=[bpq])
                    bcol = col("b_q", ch) if ch < 8 else col("b_k", ch - 8)
                    fw.op("act", lambda e: e.activation(out=raw[r][:, 0:3], in_=halo[:, ch, :], func=AF.Copy), reads=[bhalo], writes=[braw[r]])
                    fw.op("act", lambda e: e.activation(out=raw[r][:, 3:3 + TB], in_=pq, func=AF.Identity, bias=bcol, scale=1.0), reads=[bpq, bpc], writes=[braw[r]])
                    fw.op("act", lambda e: e.activation(out=halo[:, ch, :], in_=raw[r][:, TB:TB + 3], func=AF.Copy), reads=[braw[r]], writes=[bhalo])
                    wq = lambda j: pc[:, PCOL["qk_w"] + ch * 4 + j:PCOL["qk_w"] + ch * 4 + j + 1]
                    fw.op("dve", lambda e: e.tensor_scalar(out=acc[r], in0=raw[r][:, 0:TB], scalar1=wq(0), scalar2=col("qk_b", ch), op0=ALU.mult, op1=ALU.add),
                          reads=[braw[r], bpc], writes=[bacc[r]])
                    for j in range(1, 4):
                        fw.op("dve", lambda e, j=j: e.scalar_tensor_tensor(out=acc[r], in0=raw[r][:, j:j + TB], scalar=wq(j), in1=acc[r], op0=ALU.mult, op1=ALU.add),
                              reads=[braw[r], bacc[r], bpc], writes=[bacc[r]])
                    fw.op("act", lambda e: e.activation(out=qkT[:, ch, :], in_=acc[r], func=AF.Silu), reads=[bacc[r]], writes=[bqk])
                for ch in range(KC):
                    r = ch % 2
                    po, bpo = bk[r]
                    for kc in range(KC):
                        fw.op("pe", lambda e, kc=kc: e.matmul(po, lhsT=Wm[:, kc, 3072 + ch * P:3072 + (ch + 1) * P], rhs=xT[i][:, kc, :], start=(kc == 0), stop=(kc == KC - 1)),
                              reads=[bW, bxT[i]], writes=[bpo])
                    fw.op("act", lambda e: e.activation(out=soT[:, ch, :], in_=po, func=AF.Sigmoid, bias=col("b_o", ch), scale=1.0), reads=[bpo, bpc], writes=[bso])
                for j4 in range(4):
                    t0 = j4 * P; vi = j4 % 2
                    xs = lambda kc: xT[i][:, kc, t0:t0 + P]
                    for hf in range(2):
                        pv, bpv = bk[hf]
                        for kc in range(KC):
                            fw.op("pe", lambda e, kc=kc: e.matmul(pv, lhsT=xs(kc), rhs=Wm[:, kc, 2048 + hf * 512:2048 + (hf + 1) * 512], start=(kc == 0), stop=False),
                                  reads=[bW, bxT[i]], writes=[bpv])
                        fw.op("pe", lambda e: e.matmul(pv, lhsT=c["ones_bf"][0:1, :], rhs=pr[0:1, PROW["b_v"] + hf * 512:PROW["b_v"] + (hf + 1) * 512], start=False, stop=True),
                              reads=[cb, bpr], writes=[bpv])
                        fw.op("act", lambda e: e.activation(out=vp[vi][:, 2 * hf:2 * hf + 2, 0:256], in_=pv.rearrange("p (h d) -> p h d", h=2), func=AF.Copy),
                              reads=[bpv], writes=[bvp[vi]])
                    pgt, bpgt = bk[2]
                    for kc in range(KC):
                        fw.op("pe", lambda e, kc=kc: e.matmul(pgt[:, 0:8], lhsT=xs(kc), rhs=Wm[:, kc, 4096:4104], start=(kc == 0), stop=False), reads=[bW, bxT[i]], writes=[bpgt])
                    fw.op("pe", lambda e: e.matmul(pgt[:, 0:8], lhsT=c["ones_bf"][0:1, :], rhs=pr[0:1, PROW["b_if"]:PROW["b_if"] + 8], start=False, stop=True),
                          reads=[cb, bpr], writes=[bpgt])
                    fw.op("act", lambda e: e.activation(out=ifs, in_=pgt[:, 0:8], func=AF.Copy), reads=[bpgt], writes=[bg])
                    fw.op("act", lambda e: e.activation(out=ef, in_=ifs[:, 4:8], func=AF.Exp, scale=-1.0), reads=[bg], writes=[bg])
                    fw.op("act", lambda e: e.activation(out=lfn, in_=ef, func=AF.Ln, bias=c["one"], scale=1.0), reads=[bg], writes=[bg])
                    fw.op("pe", lambda e: e.matmul(pgt[:, 8:12], lhsT=c["tri_incl"], rhs=lfn, start=True, stop=True), reads=[cb, bg], writes=[bpgt])
                    fw.op("dve", lambda e: e.tensor_tensor(out=arg, in0=ifs[:, 0:4], in1=pgt[:, 8:12], op=ALU.add), reads=[bg, bpgt], writes=[bg])
                    fw.op("act", lambda e: e.activation(out=wk, in_=arg, func=AF.Exp, bias=c["nln16"], scale=1.0), reads=[bg], writes=[bg])
                    for h in range(4):
                        fw.op("dve", lambda e, h=h: e.tensor_scalar(out=lfb[:, h, :], in0=c["ones_f"], scalar1=lfn[:, h:h + 1], scalar2=None, op0=ALU.mult), reads=[bg, cb], writes=[blfb])
                    pbt, bpbt = bk[3]
                    for h in range(4):
                        fw.op("pe", lambda e, h=h: e.matmul(pbt[:, h * P:(h + 1) * P], lhsT=lfb[:, h, :], rhs=c["tri_incl"], start=True, stop=True), reads=[blfb, cb], writes=[bpbt])
                    fw.op("act", lambda e: e.activation(out=E, in_=pbt, func=AF.Exp, scale=-1.0), reads=[bpbt], writes=[bE])
                    def bset(h):
                        return (bk[4], bk[5], bk[6], bk[7]) if h % 2 == 0 else (bk[0], bk[1], bk[2], bk[3])
                    for h in range(4):
                        (pat, bpat), (ptr, bptr), _, _ = bset(h); ptrb = ptr.bitcast(BF16)
                        for dc in range(2):
                            fw.op("dve", lambda e, dc=dc: e.tensor_tensor(out=qtl[h][:, dc, :], in0=qkT[:, 2 * h + dc, t0:t0 + P], in1=E[:, h * P:(h + 1) * P], op=ALU.mult),
                                  reads=[bqk, bE], writes=[bqtl[h]])
                        for dc in range(2):
                            fw.op("pe", lambda e, dc=dc: e.matmul(pat[:, 0:P], lhsT=qkT[:, 8 + 2 * h + dc, t0:t0 + P], rhs=qtl[h][:, dc, :], start=(dc == 0), stop=(dc == 1)),
                                  reads=[bqk, bqtl[h]], writes=[bpat])
                        for dc in range(2):
                            fw.op("pe", lambda e, dc=dc: e.transpose(out=ptrb[:, dc * P:(dc + 1) * P], in_=qkT[:, 8 + 2 * h + dc, t0:t0 + P], identity=c["ident_bf"]),
                                  reads=[bqk, cb], writes=[bptr])
                        fw.op("dve", lambda e: e.scalar_tensor_tensor(out=AT[h], in0=pat[:, 0:P], scalar=wk[:, h:h + 1], in1=c["tri_incl"], op0=ALU.mult, op1=ALU.mult),
                              reads=[bpat, bg, cb], writes=[bAT[h]])
                        fw.op("act", lambda e: e.activation(out=ktok[h], in_=ptrb[:, 0:256], func=AF.Identity, scale=wk[:, h:h + 1]), reads=[bptr, bg], writes=[bktok[h]])
                    for h in range(4):
                        (pat, bpat), (ptr, bptr), (pn, bpn), (pst0, bpst0) = bset(h)
                        eL = E[:, h * P + P - 1:h * P + P]
                        fw.op("pe", lambda e: e.matmul(pn[:, 0:257], lhsT=AT[h], rhs=vp[vi][:, h, :], start=True, stop=False), reads=[bAT[h], bvp[vi]], writes=[bpn])
                        for dc in range(2):
                            fw.op("pe", lambda e, dc=dc: e.matmul(pn[:, 0:257], lhsT=qtl[h][:, dc, :], rhs=Cb[:, h, dc, :], start=False, stop=(dc == 1)), reads=[bqtl[h], bC[h]], writes=[bpn])
                        for dc in range(2):
                            pst, bpst = (pst0, bpst0) if dc == 0 else (pat, bpat)
                            o0 = 0 if dc == 0 else 128
                            fw.op("pe", lambda e, dc=dc: e.matmul(pst[:, o0:o0 + 257], lhsT=ktok[h][:, dc * P:(dc + 1) * P], rhs=vp[vi][:, h, :], start=True, stop=True),
                                  reads=[bktok[h], bvp[vi]], writes=[bpst])
                        smh = sm[h]; bsmh = bsm[h]
                        fw.op("act", lambda e: e.activation(out=smh[:, 0:1], in_=pn[:, 256:257], func=AF.Abs), reads=[bpn], writes=[bsmh])
                        fw.op("dve", lambda e: e.tensor_scalar(out=smh[:, 0:1], in0=smh[:, 0:1], scalar1=1.0, scalar2=None, op0=ALU.max), reads=[bsmh], writes=[bsmh])
                        fw.op("dve", lambda e: e.reciprocal(out=smh[:, 1:2], in_=smh[:, 0:1]), reads=[bsmh], writes=[bsmh])
                        fw.op("act", lambda e: e.activation(out=hh[h], in_=pn[:, 0:256], func=AF.Identity, scale=smh[:, 1:2]), reads=[bpn, bsmh], writes=[bhh[h]])
                        fw.op("dve", lambda e: e.bn_stats(out=smh[:, 2:8], in_=hh[h]), reads=[bhh[h]], writes=[bsmh])
                        fw.op("dve", lambda e: e.bn_aggr(out=smh[:, 8:10], in_=smh[:, 2:8]), reads=[bsmh], writes=[bsmh])
                        fw.op("act", lambda e: e.activation(out=smh[:, 10:11], in_=smh[:, 9:10], func=AF.Sqrt, bias=c["eps"], scale=1.0), reads=[bsmh], writes=[bsmh])
                        fw.op("dve", lambda e: e.reciprocal(out=smh[:, 11:12], in_=smh[:, 10:11]), reads=[bsmh], writes=[bsmh])
                        fw.op("dve", lambda e: e.tensor_scalar(out=hn[h], in0=hh[h], scalar1=smh[:, 8:9], scalar2=smh[:, 11:12], op0=ALU.subtract, op1=ALU.mult), reads=[bhh[h], bsmh], writes=[bhn[h]])
                        for dc in range(2):
                            pst, bpst = (pst0, bpst0) if dc == 0 else (pat, bpat)
                            o0 = 0 if dc == 0 else 128
                            sti = (2 * h + dc) % 4
                            fw.op("act", lambda e, dc=dc: e.activation(out=stt[sti], in_=pst[:, o0:o0 + 257], func=AF.Identity, scale=eL), reads=[bpst, bE], writes=[bstt[sti]])
                            fw.op("dve", lambda e, dc=dc: e.scalar_tensor_tensor(out=C[:, h, dc, :], in0=C[:, h, dc, :], scalar=eL, in1=stt[sti], op0=ALU.mult, op1=ALU.add),
                                  reads=[bstt[sti], bC[h], bE], writes=[bC[h]])
                            fw.op("act", lambda e, dc=dc: e.activation(out=Cb[:, h, dc, :], in_=C[:, h, dc, :], func=AF.Copy), reads=[bC[h]], writes=[bC[h]])
                    for h in range(4):
                        _, (ptr, bptr), _, _ = bset(h); ptrb = ptr.bitcast(BF16)
                        for dc in range(2):
                            fw.op("pe", lambda e, dc=dc: e.transpose(out=ptrb[:, 256 + dc * P:256 + (dc + 1) * P], in_=hn[h][:, dc * P:(dc + 1) * P], identity=c["ident_bf"]),
                                  reads=[bhn[h], cb], writes=[bptr])
                        for dc in range(2):
                            fw.op("dve", lambda e, dc=dc: e.scalar_tensor_tensor(out=hgt[i][:, 2 * h + dc, t0:t0 + P], in0=ptrb[:, 256 + dc * P:256 + (dc + 1) * P],
                                                                                   scalar=col("mn_g", 2 * h + dc), in1=soT[:, 2 * h + dc, t0:t0 + P], op0=ALU.mult, op1=ALU.mult),
                                  reads=[bptr, bpc, bso], writes=[bhgt[i]])
                fw.dma("sp", f"m_st{i}", lambda e: e.dma_start(out=self.HGT[b], in_=hgt[i]), reads=[bhgt[i]], writes=[self.DB("HGT")])
            fw.barrier()

    def pass_m2(self, l, x_src, xkey):
        fw = self.fw; c = self.c; cb = c["b"]
        with ExitStack() as st:
            sb = lambda n, s, d: self.sb(st, n, s, d)
            bk = self.banks(st)
            Wgb = sb("n_Wgb", [P, KC, D], BF16); Wmo = sb("n_Wmo", [P, KC, D], BF16); Wwo = sb("n_Wwo", [P, KC, D], BF16); bW = Buf("n_W")
            self.load_w(Wgb, self.g_win[l], OFF_GB, OFF_GB + D, self.DB(f"g_win{l}"), bW, "wl0")
            self.load_w(Wmo, self.g_sm[l][1], 0, D, self.DB(f"g_sm{l}_1"), bW, "wl1")
            self.load_w(Wwo, self.g_sm[l][2], 0, D, self.DB(f"g_sm{l}_2"), bW, "wl2")
            rw = sb("n_rw", [P, KC, NE], F32); brw = Buf("n_rw")
            fw.dma("sp", "n_rw", lambda e: e.dma_start(out=rw, in_=self.rw[l].rearrange("(kc p) e -> p kc e", p=P)), writes=[brw])
            G1 = sb("n_G1", [P, D], F32); B1 = sb("n_B1", [P, D], F32); bGB = Buf("n_GB")
            fw.dma("sp", "n_g1", lambda e: e.dma_start(out=G1, in_=self.pbc[l, 0, :].partition_broadcast(P)), writes=[bGB])
            fw.dma("sp", "n_b1", lambda e: e.dma_start(out=B1, in_=self.pbc[l, 1, :].partition_broadcast(P)), writes=[bGB])
            pc = c["pcol"]; bpc = c["bpcol"]; prf = c["prow_f"]; bpr = c["bprow"]
            col = lambda nm, i: pc[:, PCOL[nm] + i:PCOL[nm] + i + 1]
            xT = [sb(f"n_xT{i}", [P, KC, TB], BF16) for i in range(2)]; bxT = [Buf("n_xT0"), Buf("n_xT1")]
            hg = [sb(f"n_hg{i}", [P, KC, TB], BF16) for i in range(2)]; bhg = [Buf("n_hg0"), Buf("n_hg1")]
            gc = [sb(f"n_gc{i}", [P, KC, TB], BF16) for i in range(2)]; bgc = [Buf("n_gc0"), Buf("n_gc1")]
            sgb = [sb(f"n_sgb{i}", [P, TB], F32) for i in range(2)]; bsgb = [Buf("n_sgb0"), Buf("n_sgb1")]
            mix = sb("n_mix", [P, KC, TB], BF16); bmix = Buf("n_mix")
            xt = [sb(f"n_xt{i}", [P, D], F32) for i in range(2)]; bxt = [Buf("n_xt0"), Buf("n_xt1")]
            x1 = [sb(f"n_x1{i}", [P, D], F32) for i in range(2)]; bx1 = [Buf("n_x10"), Buf("n_x11")]
            x1b = [sb(f"n_x1b{i}", [P, D], BF16) for i in range(2)]; bx1b = [Buf("n_x1b0"), Buf("n_x1b1")]
            x1T = sb("n_x1T", [P, KC, P], F32); bx1T = Buf("n_x1T")
            st6 = sb("n_st6", [P, 2, 6], F32); mv = sb("n_mv", [P, 2], F32); sd = sb("n_sd", [P, 1], F32); rs = sb("n_rs", [P, 1], F32); bln = Buf("n_ln")
            lg = sb("n_lg", [P, NE], F32); v8 = sb("n_v8", [P, 8], F32); i8 = sb("n_i8", [P, 8], U32); i8f = sb("n_i8f", [P, 8], F32)
            e4 = sb("n_e4", [P, 4], F32); sm = sb("n_sm", [P, 8], F32); M = sb("n_M", [P, NE], F32); RC = sb("n_RC", [P, NE], F32); ovf = sb("n_ovf", [P, NE], F32)
            scr = sb("n_scr", [P, NE], F32); destf = sb("n_destf", [P, 4], F32); gm = sb("n_gm", [P, 4], F32); brt = Buf("n_rt")
            cnt = sb("n_cnt", [1, NE], F32); bcnt = Buf("n_cnt")
            fw.op("pool", lambda e: e.memset(cnt, 0.0), writes=[bcnt])
            dest = c["dest"]; gate = c["gate"]; bdest = c["bdest"]
            def ldb(b):
                i = b % 2
                fw.dma("sp", f"n_ldx{i}", lambda e: e.dma_start(out=xT[i], in_=self.XT[b]), reads=[self.DB("XT")], writes=[bxT[i]])
                fw.dma("sp", f"n_ldh{i}", lambda e: e.dma_start(out=hg[i], in_=self.HGT[b]), reads=[self.DB("HGT")], writes=[bhg[i]])
                fw.dma("sp", f"n_ldg{i}", lambda e: e.dma_start(out=gc[i], in_=self.GCT[b]), reads=[self.DB("GCT")], writes=[bgc[i]])
            def ldt(t):
                fw.dma("sp", f"n_ldt{t % 2}", lambda e: e.dma_start(out=xt[t % 2], in_=x_src[t * P:(t + 1) * P, :]), reads=[self.DB(xkey)], writes=[bxt[t % 2]])
            ldb(0); ldt(0)
            for b in range(NBLK):
                i = b % 2
                if b + 1 < NBLK: ldb(b + 1)
                for d in range(KC):
                    r = d % 2
                    py, bpy = bk[2 * r]; pgb, bpgb = bk[2 * r + 1]
                    for ch in range(KC):
                        fw.op("pe", lambda e, ch=ch: e.matmul(py, lhsT=Wmo[:, ch, d * P:(d + 1) * P], rhs=hg[i][:, ch, :], start=(ch == 0), stop=(ch == KC - 1)), reads=[bW, bhg[i]], writes=[bpy])
                    for kc in range(KC):
                        fw.op("pe", lambda e, kc=kc: e.matmul(pgb, lhsT=Wgb[:, kc, d * P:(d + 1) * P], rhs=xT[i][:, kc, :], start=(kc == 0), stop=(kc == KC - 1)), reads=[bW, bxT[i]], writes=[bpgb])
                    fw.op("act", lambda e: e.activation(out=sgb[r], in_=pgb, func=AF.Sigmoid, bias=col("b_gb", d), scale=1.0), reads=[bpgb, bpc], writes=[bsgb[r]])
                    fw.op("dve", lambda e: e.tensor_tensor(out=sgb[r], in0=py, in1=sgb[r], op=ALU.mult), reads=[bpy, bsgb[r]], writes=[bsgb[r]])
                    fw.op("dve", lambda e: e.tensor_tensor(out=mix[:, d, :], in0=sgb[r], in1=gc[i][:, d, :], op=ALU.add), reads=[bsgb[r], bgc[i]], writes=[bmix])
                for j4 in range(4):
                    t = b * 4 + j4; t0 = j4 * P; ti = t % 2
                    if t + 1 < NTILE: ldt(t + 1)
                    for hf in range(2):
                        po, bpo = bk[4 + hf]
                        for d in range(KC):
                            fw.op("pe", lambda e, d=d: e.matmul(po, lhsT=mix[:, d, t0:t0 + P], rhs=Wwo[:, d, hf * 512:(hf + 1) * 512], start=(d == 0), stop=(d == KC - 1)), reads=[bW, bmix], writes=[bpo])
                        fw.op("dve", lambda e, hf=hf: e.scalar_tensor_tensor(out=xt[ti][:, hf * 512:(hf + 1) * 512], in0=xt[ti][:, hf * 512:(hf + 1) * 512], scalar=ALPHA, in1=po, op0=ALU.mult, op1=ALU.add),
                              reads=[bxt[ti], bpo], writes=[bxt[ti]])
                    self.ln_tok(xt[ti], x1[ti], G1, B1, st6, mv, sd, rs, [bxt[ti]], [bln, bx1[ti]], bGB)
                    fw.dma("sp", f"n_stx{ti}", lambda e: e.dma_start(out=self.X1[t * P:(t + 1) * P, :], in_=x1[ti]), reads=[bx1[ti]], writes=[self.DB("X1")])
                    fw.op("act", lambda e: e.activation(out=x1b[ti], in_=x1[ti], func=AF.Copy), reads=[bx1[ti]], writes=[bx1b[ti]])
                    for hf in range(2):
                        ptr, bptr = bk[6 + hf]
                        for q in range(4):
                            kc = hf * 4 + q
                            fw.op("pe", lambda e, kc=kc, q=q: e.transpose(out=ptr[:, q * P:(q + 1) * P], in_=x1[ti][:, kc * P:(kc + 1) * P], identity=c["ident_f"]), reads=[bx1[ti], cb], writes=[bptr])
                        fw.op("act", lambda e, hf=hf: e.activation(out=x1T[:, hf * 4:hf * 4 + 4, :], in_=ptr.rearrange("p (k t) -> p k t", k=4), func=AF.Copy), reads=[bptr], writes=[bx1T])
                    plg, bplg = bk[0]
                    for kc in range(KC):
                        fw.op("pe", lambda e, kc=kc: e.matmul(plg[:, 0:NE], lhsT=x1T[:, kc, :], rhs=rw[:, kc, :], start=(kc == 0), stop=False), reads=[bx1T, brw], writes=[bplg])
                    fw.op("pe", lambda e: e.matmul(plg[:, 0:NE], lhsT=c["ones_f"][0:1, :], rhs=prf[0:1, 0:NE], start=False, stop=True), reads=[cb, bpr], writes=[bplg])
                    fw.op("act", lambda e: e.activation(out=lg, in_=plg[:, 0:NE], func=AF.Copy), reads=[bplg], writes=[brt])
                    fw.op("dve", lambda e: e.max(out=v8, in_=lg), reads=[brt], writes=[brt])
                    fw.op("dve", lambda e: e.max_index(out=i8, in_max=v8, in_values=lg), reads=[brt], writes=[brt])
                    fw.op("dve", lambda e: e.tensor_copy(out=i8f, in_=i8), reads=[brt], writes=[brt])
                    fw.op("dve", lambda e: e.tensor_scalar(out=sm[:, 0:1], in0=v8[:, 0:1], scalar1=-1.0, scalar2=None, op0=ALU.mult), reads=[brt], writes=[brt])
                    fw.op("act", lambda e: e.activation(out=e4, in_=v8[:, 0:4], func=AF.Exp, bias=sm[:, 0:1], scale=1.0), reads=[brt], writes=[brt])
                    fw.op("dve", lambda e: e.tensor_reduce(out=sm[:, 1:2], in_=e4, axis=mybir.AxisListType.X, op=ALU.add), reads=[brt], writes=[brt])
                    fw.op("dve", lambda e: e.reciprocal(out=sm[:, 2:3], in_=sm[:, 1:2]), reads=[brt], writes=[brt])
                    fw.op("dve", lambda e: e.tensor_scalar(out=M, in0=lg, scalar1=v8[:, 3:4], scalar2=None, op0=ALU.is_ge), reads=[brt], writes=[brt])
                    pR, bpR = bk[1]
                    fw.op("pe", lambda e: e.matmul(pR[:, 0:NE], lhsT=c["tri_strict"], rhs=M, start=True, stop=False), reads=[cb, brt], writes=[bpR])
                    fw.op("pe", lambda e: e.matmul(pR[:, 0:NE], lhsT=c["ones_f"][0:1, :], rhs=cnt, start=False, stop=True), reads=[cb, bcnt], writes=[bpR])
                    fw.op("dve", lambda e: e.tensor_scalar(out=ovf, in0=pR[:, 0:NE], scalar1=float(CAP), scalar2=1.0e7, op0=ALU.is_ge, op1=ALU.mult), reads=[bpR], writes=[brt])
                    fw.op("dve", lambda e: e.tensor_tensor(out=RC, in0=pR[:, 0:NE], in1=c["ecap"], op=ALU.add), reads=[bpR, cb], writes=[brt])
                    fw.op("dve", lambda e: e.tensor_tensor(out=RC, in0=RC, in1=ovf, op=ALU.add), reads=[brt], writes=[brt])
                    for k in range(TOPK):
                        fw.op("dve", lambda e, k=k: e.scalar_tensor_tensor(out=scr, in0=c["iota_e"], scalar=i8f[:, k:k + 1], in1=RC, op0=ALU.is_equal, op1=ALU.mult),
                              reads=[brt, cb], writes=[brt])
                        fw.op("dve", lambda e, k=k: e.tensor_reduce(out=destf[:, k:k + 1], in_=scr, axis=mybir.AxisListType.X, op=ALU.add), reads=[brt], writes=[brt])
                    fw.op("dve", lambda e: e.tensor_scalar(out=gm, in0=destf, scalar1=float(NSLOT), scalar2=None, op0=ALU.is_lt), reads=[brt], writes=[brt])
                    fw.op("dve", lambda e: e.tensor_scalar(out=destf, in0=destf, scalar1=float(NSLOT), scalar2=None, op0=ALU.min), reads=[brt], writes=[brt])
                    fw.op("dve", lambda e: e.tensor_copy(out=dest[:, t * 4:t * 4 + 4], in_=destf), reads=[brt], writes=[bdest])
                    fw.op("dve", lambda e: e.tensor_scalar(out=e4, in0=e4, scalar1=sm[:, 2:3], scalar2=None, op0=ALU.mult), reads=[brt], writes=[brt])
                    fw.op("dve", lambda e: e.tensor_tensor(out=gate[:, t * 4:t * 4 + 4], in0=e4, in1=gm, op=ALU.mult), reads=[brt], writes=[bdest])
                    pcs, bpcs = bk[2]
                    fw.op("pe", lambda e: e.matmul(pcs[0:1, 0:NE], lhsT=c["ones_f"][:, 0:1], rhs=M, start=True, stop=True), reads=[cb, brt], writes=[bpcs])
                    fw.op("dve", lambda e: e.tensor_tensor(out=cnt, in0=cnt, in1=pcs[0:1, 0:NE], op=ALU.add), reads=[bcnt, bpcs], writes=[bcnt])
                    for k in range(TOPK):
                        fw.dma("pool", f"n_sc{ti}{k}", lambda e, k=k: e.indirect_dma_start(out=self.XS, out_offset=bass.IndirectOffsetOnAxis(ap=dest[:, t * 4 + k:t * 4 + k + 1], axis=0),
                                                                                          in_=x1b[ti], in_offset=None, bounds_check=c["r_sc"], oob_is_err=False),
                               reads=[bx1b[ti], bdest])
            if self.debug:
                fw.dma("sp", "dbg0", lambda e: e.dma_start(out=self.dbg_dest, in_=dest), reads=[bdest])
                fw.dma("sp", "dbg1", lambda e: e.dma_start(out=self.dbg_gate, in_=gate), reads=[bdest])
                fw.dma("sp", "dbg2", lambda e: e.dma_start(out=self.dbg_cnt, in_=cnt), reads=[bcnt])
            fw.barrier()

    def pass_e(self, l):
        fw = self.fw; c = self.c; cb = c["b"]
        with ExitStack() as st:
            sb = lambda n, s, d: self.sb(st, n, s, d)
            bk = self.banks(st)
            W1 = [sb(f"e_W1{i}", [P, KC, 2 * DFF], BF16) for i in range(2)]
            W2 = [sb(f"e_W2{i}", [P, KC, D], BF16) for i in range(2)]
            b2r = [sb(f"e_b2{i}", [1, D], BF16) for i in range(2)]
            bW = [Buf("e_W0"), Buf("e_W1")]
            pc = c["pcol"]; bpc = c["bpcol"]
            xs = [sb(f"e_xs{i}", [P, 4, D], BF16) for i in range(2)]; bxs = [Buf("e_xs0"), Buf("e_xs1")]
            XTt = sb("e_XT", [P, KC, TB], BF16); bXT = Buf("e_XT")
            gcl = [sb(f"e_gc{i}", [P, TB], F32) for i in range(2)]; sgl = [sb(f"e_sg{i}", [P, TB], F32) for i in range(2)]; lcl = [sb(f"e_lc{i}", [P, TB], F32) for i in range(2)]
            bgl = [Buf("e_g0"), Buf("e_g1")]
            actT = sb("e_act", [P, KC, TB], BF16); bact = Buf("e_act")
            ys = [sb(f"e_ys{i}", [P, D], BF16) for i in range(2)]; bys = [Buf("e_ys0"), Buf("e_ys1")]

            def loadw(e_):
                i = e_ % 2
                src1 = self.g_w1[l][e_ * D:(e_ + 1) * D, :]
                src2 = self.g_w2[l][e_ * DFF:(e_ + 1) * DFF, :]
                self.load_w(W1[i], src1, 0, 2 * DFF, self.DB(f"g_w1{l}"), bW[i], f"e_w1{i}")
                self.load_w(W2[i], src2, 0, D, self.DB(f"g_w2{l}"), bW[i], f"e_w2{i}")
                fw.dma("pool", f"e_b2{i}", lambda e: e.dma_start(out=b2r[i], in_=self.prow[l, 0:1, PROW["b2"] + e_ * D:PROW["b2"] + (e_ + 1) * D]), writes=[bW[i]])

            loadw(0)
            blist = [(e_, boff, bw) for e_ in range(NE) for (boff, bw) in EBLK]
            def ldx(n):
                e2, bo2, bw2 = blist[n]; s2 = e2 * CAP + bo2
                fw.dma("sp", f"e_ld{n % 2}", lambda e: e.dma_start(out=xs[n % 2][:, 0:bw2 // P, :], in_=self.XS[s2:s2 + bw2, :].rearrange("(j p) d -> p j d", p=P)), writes=[bxs[n % 2]])
            ldx(0)
            nb = 0
            for e_ in range(NE):
                wi = e_ % 2
                if e_ + 1 < NE:
                    loadw(e_ + 1)
                for (boff, bw) in EBLK:
                    s0 = e_ * CAP + boff; xi = nb % 2; nb += 1; nj = bw // P
                    if nb < len(blist): ldx(nb)
                    for j in range(nj):
                        ptr, bptr = bk[6 + (j % 2)]; pv = ptr.bitcast(BF16)
                        for kc in range(KC):
                            fw.op("pe", lambda e, kc=kc: e.transpose(out=pv[:, kc * P:(kc + 1) * P], in_=xs[xi][:, j, kc * P:(kc + 1) * P], identity=c["ident_bf"]), reads=[bxs[xi], cb], writes=[bptr])
                        fw.op("act", lambda e: e.activation(out=XTt[:, :, j * P:(j + 1) * P], in_=pv.rearrange("p (k t) -> p k t", k=KC), func=AF.Copy), reads=[bptr], writes=[bXT])
                    for ch in range(KC):
                        r = ch % 2
                        pg, bpg = bk[2 * r]; pl, bpl = bk[2 * r + 1]
                        for kc in range(KC):
                            fw.op("pe", lambda e, kc=kc: e.matmul(pg[:, 0:bw], lhsT=W1[wi][:, kc, ch * P:(ch + 1) * P], rhs=XTt[:, kc, 0:bw], start=(kc == 0), stop=(kc == KC - 1)), reads=[bW[wi], bXT], writes=[bpg])
                        for kc in range(KC):
                            fw.op("pe", lambda e, kc=kc: e.matmul(pl[:, 0:bw], lhsT=W1[wi][:, kc, DFF + ch * P:DFF + (ch + 1) * P], rhs=XTt[:, kc, 0:bw], start=(kc == 0), stop=(kc == KC - 1)), reads=[bW[wi], bXT], writes=[bpl])
                        b1g = pc[:, PCOL["b1"] + e_ * 16 + ch:PCOL["b1"] + e_ * 16 + ch + 1]
                        b1l = pc[:, PCOL["b1"] + e_ * 16 + 8 + ch:PCOL["b1"] + e_ * 16 + 8 + ch + 1]
                        fw.op("dve", lambda e: e.tensor_scalar(out=gcl[r][:, 0:bw], in0=pg[:, 0:bw], scalar1=b1g, scalar2=LIMIT, op0=ALU.add, op1=ALU.min), reads=[bpg, bpc], writes=[bgl[r]])
                        fw.op("act", lambda e: e.activation(out=sgl[r][:, 0:bw], in_=gcl[r][:, 0:bw], func=AF.Sigmoid, scale=SW_ALPHA), reads=[bgl[r]], writes=[bgl[r]])
                        fw.op("dve", lambda e: e.tensor_scalar(out=lcl[r][:, 0:bw], in0=pl[:, 0:bw], scalar1=b1l, scalar2=LIMIT, op0=ALU.add, op1=ALU.min), reads=[bpl, bpc], writes=[bgl[r]])
                        fw.op("dve", lambda e: e.tensor_scalar(out=lcl[r][:, 0:bw], in0=lcl[r][:, 0:bw], scalar1=-LIMIT, scalar2=1.0, op0=ALU.max, op1=ALU.add), reads=[bgl[r]], writes=[bgl[r]])
                        fw.op("pool", lambda e: e.tensor_tensor(out=gcl[r][:, 0:bw], in0=gcl[r][:, 0:bw], in1=sgl[r][:, 0:bw], op=ALU.mult), reads=[bgl[r]], writes=[bgl[r]])
                        fw.op("pool", lambda e: e.tensor_tensor(out=actT[:, ch, 0:bw], in0=gcl[r][:, 0:bw], in1=lcl[r][:, 0:bw], op=ALU.mult), reads=[bgl[r]], writes=[bact])
                    for j in range(nj):
                        yi = j % 2
                        for hf in range(2):
                            py, bpy = bk[4 + hf]
                            for ch in range(KC):
                                fw.op("pe", lambda e, ch=ch: e.matmul(py, lhsT=actT[:, ch, j * P:(j + 1) * P], rhs=W2[wi][:, ch, hf * 512:(hf + 1) * 512], start=(ch == 0), stop=False), reads=[bact, bW[wi]], writes=[bpy])
                            fw.op("pe", lambda e: e.matmul(py, lhsT=c["ones_bf"][0:1, :], rhs=b2r[wi][0:1, hf * 512:(hf + 1) * 512], start=False, stop=True), reads=[cb, bW[wi]], writes=[bpy])
                            fw.op("act", lambda e, hf=hf: e.activation(out=ys[yi][:, hf * 512:(hf + 1) * 512], in_=py, func=AF.Copy), reads=[bpy], writes=[bys[yi]])
                        fw.dma("sp", f"e_st{yi}", lambda e: e.dma_start(out=self.YS[s0 + j * P:s0 + (j + 1) * P, :], in_=ys[yi]), reads=[bys[yi]])
            fw.barrier()

    def pass_f(self, l, dst, dkey, make_xt):
        fw = self.fw; c = self.c; cb = c["b"]
        with ExitStack() as st:
            sb = lambda n, s, d: self.sb(st, n, s, d)
            bk = self.banks(st)
            G2 = sb("f_G2", [P, D], F32); B2 = sb("f_B2", [P, D], F32); bGB = Buf("f_GB")
            fw.dma("sp", "f_g2", lambda e: e.dma_start(out=G2, in_=self.pbc[l, 2, :].partition_broadcast(P)), writes=[bGB])
            fw.dma("sp", "f_b2", lambda e: e.dma_start(out=B2, in_=self.pbc[l, 3, :].partition_broadcast(P)), writes=[bGB])
            yg = [sb(f"f_yg{i}", [P, 4, D], BF16) for i in range(2)]; byg = [Buf("f_yg0"), Buf("f_yg1")]
            xt = [sb(f"f_xt{i}", [P, D], F32) for i in range(2)]; bxt = [Buf("f_xt0"), Buf("f_xt1")]
            acc = sb("f_acc", [P, D], F32); bacc = Buf("f_acc")
            xo = [sb(f"f_xo{i}", [P, D], F32) for i in range(2)]; bxo = [Buf("f_xo0"), Buf("f_xo1")]
            xb = [sb(f"f_xb{i}", [P, D], BF16) for i in range(2)]; bxb = [Buf("f_xb0"), Buf("f_xb1")]
            xT = [sb(f"f_xT{i}", [P, KC, TB], BF16) for i in range(2)]; bT = [Buf("f_T0"), Buf("f_T1")]
            st6 = sb("f_st6", [P, 2, 6], F32); mv = sb("f_mv", [P, 2], F32); sd = sb("f_sd", [P, 1], F32); rs = sb("f_rs", [P, 1], F32); bln = Buf("f_ln")
            dest = c["dest"]; gate = c["gate"]; bdest = c["bdest"]
            def ldf(t):
                i = t % 2
                for k in range(TOPK):
                    fw.dma("pool", f"f_g{i}{k}", lambda e, k=k: e.indirect_dma_start(out=yg[i][:, k, :], out_offset=None, in_=self.YS,
                                                                                     in_offset=bass.IndirectOffsetOnAxis(ap=dest[:, t * 4 + k:t * 4 + k + 1], axis=0), bounds_check=c["r_ga"], oob_is_err=False),
                           reads=[bdest], writes=[byg[i]])
                fw.dma("sp", f"f_ld{i}", lambda e: e.dma_start(out=xt[i], in_=self.X1[t * P:(t + 1) * P, :]), reads=[self.DB("X1")], writes=[bxt[i]])
            ldf(0)
            for t in range(NTILE):
                i = t % 2; blk = t // 4; j = t % 4
                if t + 1 < NTILE: ldf(t + 1)
                fw.op("dve", lambda e: e.tensor_scalar(out=acc, in0=yg[i][:, 0, :], scalar1=gate[:, t * 4:t * 4 + 1], scalar2=None, op0=ALU.mult), reads=[byg[i], bdest], writes=[bacc])
                for k in range(1, TOPK):
                    fw.op("dve", lambda e, k=k: e.scalar_tensor_tensor(out=acc, in0=yg[i][:, k, :], scalar=gate[:, t * 4 + k:t * 4 + k + 1], in1=acc, op0=ALU.mult, op1=ALU.add), reads=[byg[i], bdest, bacc], writes=[bacc])
                fw.op("dve", lambda e: e.scalar_tensor_tensor(out=xt[i], in0=xt[i], scalar=ALPHA, in1=acc, op0=ALU.mult, op1=ALU.add), reads=[bxt[i], bacc], writes=[bxt[i]])
                self.ln_tok(xt[i], xo[i], G2, B2, st6, mv, sd, rs, [bxt[i]], [bln, bxo[i]], bGB)
                fw.dma("sp", f"f_st{i}", lambda e: e.dma_start(out=dst[t * P:(t + 1) * P, :], in_=xo[i]), reads=[bxo[i]], writes=[self.DB(dkey)])
                if make_xt:
                    fw.op("act", lambda e: e.activation(out=xb[i], in_=xo[i], func=AF.Copy), reads=[bxo[i]], writes=[bxb[i]])
                    pb, pbuf = bk[t % 2]; pv = pb.bitcast(BF16)
                    for kc in range(KC):
                        fw.op("pe", lambda e, kc=kc: e.transpose(out=pv[:, kc * P:(kc + 1) * P], in_=xb[i][:, kc * P:(kc + 1) * P], identity=c["ident_bf"]), reads=[bxb[i], cb], writes=[pbuf])
                    T = xT[blk % 2]
                    fw.op("act", lambda e: e.activation(out=T[:, :, j * P:(j + 1) * P], in_=pv.rearrange("p (k t) -> p k t", k=KC), func=AF.Copy), reads=[pbuf], writes=[bT[blk % 2]])
                    if j == 3:
                        fw.dma("sp", f"f_sT{blk % 2}", lambda e: e.dma_start(out=self.XT[blk], in_=T), reads=[bT[blk % 2]], writes=[self.DB("XT")])
            fw.barrier()

    def load_params(self, st0, l):
        fw = self.fw; c = self.c
        if "pcol" not in c:
            c["pcol"] = self.sb(st0, "pcol_sb", [P, NCOL], F32); c["bpcol"] = Buf("pcol")
            c["prow_bf"] = self.sb(st0, "prow_bf", [1, 1032], BF16); c["prow_f"] = self.sb(st0, "prow_f", [1, NE], F32); c["bprow"] = Buf("prow")
            for nm, val in [("eps", EPS), ("one", 1.0), ("nln16", -math.log(16.0))]:
                c[nm] = self.sb(st0, "k_" + nm, [P, 1], F32)
                fw.op("pool", lambda e, nm=nm, val=val: e.memset(c[nm], val), writes=[c["b"]])
        fw.barrier()
        fw.dma("sp", "pcol", lambda e: e.dma_start(out=c["pcol"], in_=self.pcol[l]), writes=[c["bpcol"]])
        fw.dma("pool", "prow", lambda e: e.dma_start(out=c["prow_bf"], in_=self.prow[l, 0:1, 0:1032]), writes=[c["bprow"]])
        fw.dma("sp", "prowf", lambda e: e.dma_start(out=c["prow_f"], in_=self.prow[l, 0:1, PROW["r_b"]:PROW["r_b"] + NE]), writes=[c["bprow"]])

    def build(self):
        fw = self.fw
        with ExitStack() as st0:
            self.consts(st0)
            self.gathers(0)
            self.zero_scratch()
            self.pass_x0(self.x_in, "x_in")
            for l in range(self.Lp):
                self.load_params(st0, l)
                x_src, xkey = (self.x_in, "x_in") if l == 0 else (self.XN[(l - 1) % 2], f"XN{(l - 1) % 2}")
                last = (l == self.Lp - 1)
                dst, dkey = (self.out, "out") if last else (self.XN[l % 2], f"XN{l % 2}")
                self.pass_c(l)
                self.pass_m1(l)
                self.pass_m2(l, x_src, xkey)
                self.pass_e(l)
                self.pass_f(l, dst, dkey, make_xt=not last)
            fw.barrier()
            self.finish()
        return self.nc

    def finish(self):
        fw = self.fw; nc = self.nc
        for k in ("cc", "gcp0", "gcp1", "gcp2", "gcp3"):
            if k in fw.dsem:
                for e in fw.engs:
                    fw._wait(e, k, fw.dsem[k][1])
        fin = nc.alloc_semaphore(name="fin")
        for e in ("pe", "act", "dve", "sp"):
            fw.engs[e].sem_inc(fin, 1)
        g = nc.gpsimd
        g.wait_ge(fin, 4)
        for k in fw.sem:
            g.sem_clear(fw.sem[k])
        for k, (sh, c) in fw.dsem.items():
            g.sem_clear(sh)
        g.sem_clear(fin)


def _pcol_host(b_in, dw_w, dw_b, cn_g, cn_b, co_b, qk_w, qk_b, mn_g, b1):
    a = np.zeros((P, NCOL), np.float32)
    def cols(v, n):
        return np.ascontiguousarray(v.reshape(n, P).T)
    a[:, PCOL["b_a"]:PCOL["b_a"] + 8] = cols(b_in[0:1024], 8)
    a[:, PCOL["b_g"]:PCOL["b_g"] + 8] = cols(b_in[1024:2048], 8)
    a[:, PCOL["b_q"]:PCOL["b_q"] + 8] = cols(b_in[OFF_Q:OFF_Q + 1024], 8)
    a[:, PCOL["b_k"]:PCOL["b_k"] + 8] = cols(b_in[OFF_K:OFF_K + 1024], 8)
    a[:, PCOL["b_o"]:PCOL["b_o"] + 8] = cols(b_in[OFF_O:OFF_O + 1024], 8)
    a[:, PCOL["b_ga"]:PCOL["b_ga"] + 8] = cols(b_in[OFF_GA:OFF_GA + 1024], 8)
    a[:, PCOL["b_gb"]:PCOL["b_gb"] + 8] = cols(b_in[OFF_GB:OFF_GB + 1024], 8)
    a[:, PCOL["dw_w"]:PCOL["dw_w"] + 8 * TAPS] = dw_w.reshape(TAPS, 8, P).transpose(2, 1, 0).reshape(P, 8 * TAPS)
    a[:, PCOL["dw_b"]:PCOL["dw_b"] + 8] = cols(dw_b, 8)
    a[:, PCOL["cn_g"]:PCOL["cn_g"] + 8] = cols(cn_g, 8)
    a[:, PCOL["cn_b"]:PCOL["cn_b"] + 8] = cols(cn_b, 8)
    a[:, PCOL["co_b"]:PCOL["co_b"] + 8] = cols(co_b, 8)
    a[:, PCOL["qk_w"]:PCOL["qk_w"] + 64] = qk_w.reshape(4, 16, P).transpose(2, 1, 0).reshape(P, 64)
    a[:, PCOL["qk_b"]:PCOL["qk_b"] + 16] = cols(qk_b, 16)
    a[:, PCOL["mn_g"]:PCOL["mn_g"] + 8] = cols(mn_g, 8)
    a[:, PCOL["b1"]:PCOL["b1"] + NE * 16] = b1.reshape(NE, 16, P).transpose(2, 0, 1).reshape(P, NE * 16)
    return a


def _prow_host(b_in, r_b, b2):
    a = np.zeros((1, NROW), np.float32)
    a[0, 0:1024] = b_in[OFF_V:OFF_V + 1024]
    a[0, 1024:1032] = b_in[OFF_I:OFF_I + 8]
    a[0, 1032:1064] = r_b
    a[0, 1064:] = b2.reshape(-1)
    return a


_NC_CACHE = {}


def _layer_inputs(ls, inp, core):
    W = lambda k: np.asarray(inp[k])
    sl = slice(core * P, (core + 1) * P)
    es = slice(core * 4, core * 4 + 4)
    d = {}
    d["w_in_sh"] = np.stack([W("w_in")[l][sl] for l in ls])
    d["co_sh"] = np.stack([W("conv_out_w")[l][sl] for l in ls])
    d["mo_sh"] = np.stack([W("mlstm_out_w")[l][sl] for l in ls])
    d["wo_sh"] = np.stack([W("w_out")[l][sl] for l in ls])
    d["w1_sh"] = np.stack([W("moe_w1")[l][es].reshape(4 * D, 2 * DFF) for l in ls])
    d["w2_sh"] = np.stack([W("moe_w2")[l][es].reshape(4 * DFF, D) for l in ls])
    d["pcol"] = np.stack([_pcol_host(W("b_in")[l], W("conv_dw_w")[l], W("conv_dw_b")[l], W("conv_norm_g")[l], W("conv_norm_b")[l], W("conv_out_b")[l],
                                     W("qk_conv_w")[l], W("qk_conv_b")[l], W("mlstm_norm_g")[l], W("moe_b1")[l]) for l in ls])
    d["prow"] = np.stack([_prow_host(W("b_in")[l], W("router_b")[l], W("moe_b2")[l]) for l in ls])
    d["pbc"] = np.stack([np.stack([W("ln1_g")[l], W("ln1_b")[l], W("ln2_g")[l], W("ln2_b")[l]]) for l in ls])
    d["rw"] = np.stack([W("router_w")[l] for l in ls])
    return d


def run_layers(x_full, inp, ls, debug=False):
    key = (len(ls), debug)
    nc = Builder(len(ls), debug=debug).build()
    xs = np.ascontiguousarray(x_full, dtype=np.float32).reshape(NCORES, NT, D)
    in_maps = []
    for c in range(NCORES):
        d = _layer_inputs(ls, inp, c)
        d["x"] = xs[c]
        in_maps.append(d)
    res = run_bass_kernel_spmd(nc, in_maps, core_ids=list(range(NCORES)))
    out = np.stack([r["out"] for r in res.results]).reshape(x_full.shape)
    return out, res


FUSED = True


def kernel(**inputs):
    x = np.asarray(inputs["x"], dtype=np.float32)
    if FUSED:
        out, _ = run_layers(x, inputs, list(range(DEPTH)))
        return out
    for l in range(DEPTH):
        x, _ = run_layers(x, inputs, [l])
    return x
```

```python
import math
from contextlib import ExitStack

import numpy as np
import concourse.bass as bass
import concourse.mybir as mybir
from concourse.bass_utils import run_bass_kernel_spmd

F32 = mybir.dt.float32; BF16 = mybir.dt.bfloat16; U32 = mybir.dt.uint32; I32 = mybir.dt.int32
AF = mybir.ActivationFunctionType; ALU = mybir.AluOpType

NCORES = 8
D = 1024; KC = 8; P = 128
NT = 8192; SEQ = 4096; TB = 512; NBLK = NT // TB; BPS = SEQ // TB; NTILE = NT // P
DEPTH = 4
NE = 32; TOPK = 4; DFF = 1024
CAP = 1280; NSLOT = NE * CAP; EBLK = [(0, 512), (512, 512), (1024, 256)]
TAPS = 31
OFF_Q = 2048; OFF_K = 3072; OFF_V = 4096; OFF_O = 5120; OFF_I = 6144; OFF_GA = 6152; OFF_GB = 7176; N_IN = 8200
ALPHA = (2 * DEPTH) ** 0.25
EPS = 1e-5
LIMIT = 7.0; SW_ALPHA = 1.702

PCOL = {}
_o = 0
for _n, _w in [("b_a", 8), ("b_g", 8), ("b_q", 8), ("b_k", 8), ("b_o", 8), ("b_ga", 8), ("b_gb", 8),
               ("dw_w", 8 * TAPS), ("dw_b", 8), ("cn_g", 8), ("cn_b", 8), ("co_b", 8),
               ("qk_w", 16 * 4), ("qk_b", 16), ("mn_g", 8), ("b1", NE * 16)]:
    PCOL[_n] = _o; _o += _w
NCOL = _o
PROW = {"b_v": 0, "b_if": 1024, "r_b": 1032, "b2": 1064}
NROW = 1064 + NE * D


class Buf:
    __slots__ = ("name", "w", "r")

    def __init__(self, name):
        self.name = name; self.w = None; self.r = []


class FW:
    def __init__(self, nc):
        self.nc = nc
        self.engs = {"pe": nc.tensor, "act": nc.scalar, "dve": nc.vector, "pool": nc.gpsimd, "sp": nc.sync}
        self.sem = {k: nc.alloc_semaphore(name=f"sem_{k}") for k in ("pe", "act", "dve", "pool")}
        self.cnt = {k: 0 for k in self.sem}
        self.seen = {k: {} for k in self.engs}
        self.dsem = {}
        self.nwait = 0; self.nins = 0

    def _semh(self, key):
        return self.sem[key] if key in self.sem else self.dsem[key][0]

    def _wait(self, eng, k, v):
        seen = self.seen[eng]
        if v <= 0 or seen.get(k, 0) >= v:
            return
        self.engs[eng].wait_ge(self._semh(k), v)
        seen[k] = v; self.nwait += 1

    def _sync(self, eng, reads, writes, extra=()):
        need = {}
        def add(ev):
            k, v = ev
            if k == eng and eng == "pe":
                return
            if v > need.get(k, 0): need[k] = v
        for ev in extra: add(ev)
        for b in reads:
            if b.w is not None: add(b.w)
        for b in writes:
            if b.w is not None and b.w[0] != eng: add(b.w)
            for ev in b.r: add(ev)
        for k, v in need.items():
            self._wait(eng, k, v)

    def _record(self, ev, reads, writes):
        for b in reads:
            b.r.append(ev)
            if len(b.r) > 24:
                m = {}
                for k, v in b.r:
                    if v > m.get(k, 0): m[k] = v
                b.r = list(m.items())
        for b in writes:
            b.w = ev; b.r = []

    def op(self, eng, fn, reads=(), writes=()):
        self._sync(eng, reads, writes)
        ins = fn(self.engs[eng])
        self.cnt[eng] += 1; self.nins += 1
        ins.then_inc(self.sem[eng], 1)
        self._record((eng, self.cnt[eng]), reads, writes)
        return ins

    def dma(self, q, key, fn, reads=(), writes=(), inc=16):
        if key not in self.dsem:
            self.dsem[key] = [self.nc.alloc_semaphore(name=f"d_{key}"), 0]
        s = self.dsem[key]
        self._sync(q, reads, writes, [(key, s[1])])
        ins = fn(self.engs[q])
        s[1] += inc; self.nins += 1
        ins.then_inc(s[0], inc)
        self._record((key, s[1]), reads, writes)
        return ins

    def barrier(self, engs=("pe", "act", "dve", "pool", "sp")):
        for e in engs:
            for k in self.sem:
                if k != e: self._wait(e, k, self.cnt[k])
            for k, (s, c) in self.dsem.items():
                if k != "cc" and not k.startswith("gcp"):
                    self._wait(e, k, c)


class Builder:
    def __init__(self, Lp, debug=False):
        self.Lp = Lp; self.debug = debug
        nc = self.nc = bass.Bass("TRN2", target_bir_lowering=False)
        self.fw = FW(nc)
        self.dbufs = {}
        dt = lambda name, shape, dtype, kind: nc.dram_tensor(name, shape, dtype, kind=kind).ap()
        EI = "ExternalInput"
        self.x_in = dt("x", [NT, D], F32, EI)
        self.out = dt("out", [NT, D], F32, "ExternalOutput")
        self.w_in_sh = dt("w_in_sh", [Lp, P, N_IN], F32, EI)
        self.co_sh = dt("co_sh", [Lp, P, D], F32, EI)
        self.mo_sh = dt("mo_sh", [Lp, P, D], F32, EI)
        self.wo_sh = dt("wo_sh", [Lp, P, D], F32, EI)
        self.w1_sh = dt("w1_sh", [Lp, 4 * D, 2 * DFF], F32, EI)
        self.w2_sh = dt("w2_sh", [Lp, 4 * DFF, D], F32, EI)
        self.pcol = dt("pcol", [Lp, P, NCOL], F32, EI)
        self.prow = dt("prow", [Lp, 1, NROW], F32, EI)
        self.pbc = dt("pbc", [Lp, 4, D], F32, EI)
        self.rw = dt("rw", [Lp, D, NE], F32, EI)
        IN = "Internal"
        dk = "ExternalOutput" if debug else IN
        self.s_win = [dt(f"s_win{l}", [P, N_IN], F32, IN) for l in range(Lp)]
        self.g_win = [dt(f"g_win{l}", [D, N_IN], F32, IN) for l in range(Lp)]
        self.s_sm = [[dt(f"s_sm{l}_{i}", [P, D], F32, IN) for i in range(3)] for l in range(Lp)]
        self.g_sm = [[dt(f"g_sm{l}_{i}", [D, D], F32, IN) for i in range(3)] for l in range(Lp)]
        self.s_w1 = [dt(f"s_w1{l}", [4 * D, 2 * DFF], F32, IN) for l in range(Lp)]
        self.g_w1 = [dt(f"g_w1{l}", [NE * D, 2 * DFF], F32, IN) for l in range(Lp)]
        self.s_w2 = [dt(f"s_w2{l}", [4 * DFF, D], F32, IN) for l in range(Lp)]
        self.g_w2 = [dt(f"g_w2{l}", [NE * DFF, D], F32, IN) for l in range(Lp)]
        self.XT = dt("XT", [NBLK, P, KC, TB], BF16, IN)
        self.GCT = dt("GCT", [NBLK, P, KC, TB], BF16, dk)
        self.HGT = dt("HGT", [NBLK, P, KC, TB], BF16, dk)
        self.X1 = dt("X1", [NT, D], F32, dk)
        self.XS = dt("XS", [NSLOT, D], BF16, IN)
        self.YS = dt("YS", [NSLOT + 1, D], BF16, IN)
        self.XN = [dt(f"XN{i}", [NT, D], F32, IN) for i in range(2)] if Lp > 1 else []
        if debug:
            self.dbg_dest = dt("dbg_dest", [P, NTILE * 4], I32, "ExternalOutput")
            self.dbg_gate = dt("dbg_gate", [P, NTILE * 4], F32, "ExternalOutput")
            self.dbg_cnt = dt("dbg_cnt", [1, NE], F32, "ExternalOutput")

    def DB(self, name):
        b = self.dbufs.get(name)
        if b is None:
            b = self.dbufs[name] = Buf(name)
        return b

    def sb(self, st, name, shape, dtype):
        self.uid = getattr(self, "uid", 0) + 1
        t = st.enter_context(self.nc.sbuf_tensor(f"{name}_u{self.uid}", shape, dtype))
        return t.ap() if hasattr(t, "ap") and callable(getattr(t, "ap")) else t

    def banks(self, st):
        res = []
        for i in range(8):
            self.uid = getattr(self, "uid", 0) + 1
            t = st.enter_context(self.nc.psum_tensor(f"bank{i}_u{self.uid}", [P, 512], F32))
            res.append((t.ap() if callable(getattr(t, "ap", None)) else t, Buf(f"bank{i}")))
        return res

    def consts(self, st):
        fw = self.fw; sb = self.sb
        c = self.c = {}
        c["b"] = Buf("consts")
        cb = c["b"]
        c["ident_bf"] = sb(st, "ident_bf", [P, P], BF16)
        c["ident_f"] = sb(st, "ident_f", [P, P], F32)
        c["ones_bf"] = sb(st, "ones_bf", [P, P], BF16)
        c["ones_f"] = sb(st, "ones_f", [P, P], F32)
        c["tri_incl"] = sb(st, "tri_incl", [P, P], F32)
        c["tri_strict"] = sb(st, "tri_strict", [P, P], F32)
        c["iota_e"] = sb(st, "iota_e", [P, NE], F32)
        c["ecap"] = sb(st, "ecap", [P, NE], F32)
        iota_i = sb(st, "iota_i", [P, NE], I32)
        c["zrow"] = sb(st, "zrow", [1, D], BF16)
        c["zt"] = sb(st, "zt", [P, 8, D], BF16)
        g = "pool"
        for nm, val in [("ident_bf", 1.0), ("ident_f", 1.0), ("ones_bf", 1.0), ("ones_f", 1.0), ("tri_incl", 1.0), ("tri_strict", 1.0), ("zrow", 0.0), ("zt", 0.0)]:
            fw.op(g, lambda e, nm=nm, val=val: e.memset(c[nm], val), writes=[cb])
        for nm, op_, base in [("ident_bf", ALU.is_equal, 0), ("ident_f", ALU.is_equal, 0), ("tri_incl", ALU.is_ge, 0), ("tri_strict", ALU.is_gt, 0)]:
            fw.op(g, lambda e, nm=nm, op_=op_, base=base: e.affine_select(out=c[nm], in_=c[nm], pattern=[[1, P]], compare_op=op_, fill=0.0, base=base, channel_multiplier=-1),
                  reads=[cb], writes=[cb])
        fw.op(g, lambda e: e.iota(iota_i, pattern=[[1, NE]], base=0, channel_multiplier=0), writes=[cb])
        fw.op("dve", lambda e: e.tensor_copy(out=c["iota_e"], in_=iota_i), reads=[cb], writes=[cb])
        fw.op("dve", lambda e: e.tensor_scalar(out=c["ecap"], in0=c["iota_e"], scalar1=float(CAP), scalar2=None, op0=ALU.mult), reads=[cb], writes=[cb])
        fw.dma("sp", "zrow", lambda e: e.dma_start(out=self.YS[NSLOT:NSLOT + 1, :], in_=c["zrow"]), reads=[cb])
        c["dest"] = sb(st, "dest", [P, NTILE * 4], I32)
        c["gate"] = sb(st, "gate", [P, NTILE * 4], F32)
        c["bdest"] = Buf("dest")
        c["r_sc"] = self.nc.gpsimd.to_reg(NSLOT - 1)
        c["r_ga"] = self.nc.gpsimd.to_reg(NSLOT)

    def gathers(self, l, parts=("win", "sm", "w1", "w2")):
        fw = self.fw
        rg = [list(range(NCORES))]
        def one(name, shard_ap, s_ap, g_ap, rows):
            CH = 1024
            for r0 in range(0, rows, CH):
                r1 = min(rows, r0 + CH)
                self.ngcp = getattr(self, "ngcp", 0) + 1
                fw.dma("sp", f"gcp{self.ngcp % 4}", lambda e: e.dma_start(out=s_ap[r0:r1, :], in_=shard_ap[r0:r1, :]), writes=[self.DB("s_" + name)])
            fw.dma("pool", "cc", lambda e: e.collective_compute("AllGather", ALU.bypass, replica_groups=rg, ins=[s_ap], outs=[g_ap]),
                   reads=[self.DB("s_" + name)], writes=[self.DB("g_" + name)], inc=1)
        if "win" in parts:
            one(f"win{l}", self.w_in_sh[l], self.s_win[l], self.g_win[l], P)
        if "sm" in parts:
            for i, sh in enumerate([self.co_sh, self.mo_sh, self.wo_sh]):
                one(f"sm{l}_{i}", sh[l], self.s_sm[l][i], self.g_sm[l][i], P)
        if "w1" in parts:
            one(f"w1{l}", self.w1_sh[l], self.s_w1[l], self.g_w1[l], 4 * D)
        if "w2" in parts:
            one(f"w2{l}", self.w2_sh[l], self.s_w2[l], self.g_w2[l], 4 * DFF)

    def load_w(self, dst, src2d, c0, c1, rbuf, wbuf, key):
        srcr = src2d.rearrange("(kc p) n -> p kc n", p=P)
        o = 0
        while c0 + o < c1:
            w = min(2048, c1 - c0 - o)
            self.fw.dma("pool", key, lambda e, o=o, w=w: e.dma_start(out=dst[:, :, o:o + w], in_=srcr[:, :, c0 + o:c0 + o + w]),
                        reads=[rbuf], writes=[wbuf])
            o += w

    def ln_tok(self, r, out, G, Bt, st6, mv, sd, rs, bufs_r, bufs_w, cbuf):
        fw = self.fw
        for hlf in range(2):
            fw.op("dve", lambda e, hlf=hlf: e.bn_stats(out=st6[:, hlf, :], in_=r[:, hlf * 512:(hlf + 1) * 512]), reads=bufs_r, writes=[bufs_w[0]])
        fw.op("dve", lambda e: e.bn_aggr(out=mv, in_=st6), reads=[bufs_w[0]], writes=[bufs_w[0]])
        fw.op("act", lambda e: e.activation(out=sd, in_=mv[:, 1:2], func=AF.Sqrt, bias=self.c["eps"], scale=1.0), reads=[bufs_w[0]], writes=[bufs_w[0]])
        fw.op("dve", lambda e: e.reciprocal(out=rs, in_=sd), reads=[bufs_w[0]], writes=[bufs_w[0]])
        fw.op("dve", lambda e: e.tensor_scalar(out=r, in0=r, scalar1=mv[:, 0:1], scalar2=rs, op0=ALU.subtract, op1=ALU.mult), reads=bufs_r + [bufs_w[0]], writes=bufs_r)
        fw.op("dve", lambda e: e.tensor_tensor(out=r, in0=r, in1=G, op=ALU.mult), reads=bufs_r + [cbuf], writes=bufs_r)
        fw.op("dve", lambda e: e.tensor_tensor(out=out, in0=r, in1=Bt, op=ALU.add), reads=bufs_r + [cbuf], writes=[bufs_w[1]])

    def pass_x0(self, x_src, xkey):
        fw = self.fw; c = self.c
        with ExitStack() as st:
            bk = self.banks(st)
            xt = [self.sb(st, f"x0_x{i}", [P, D], F32) for i in range(2)]
            xb = [self.sb(st, f"x0_xb{i}", [P, D], BF16) for i in range(2)]
            xT = [self.sb(st, f"x0_T{i}", [P, KC, TB], BF16) for i in range(2)]
            bx = [Buf("x0x0"), Buf("x0x1")]; bxb = [Buf("x0b0"), Buf("x0b1")]; bT = [Buf("x0T0"), Buf("x0T1")]
            def ld(t):
                fw.dma("sp", f"x0ld{t % 2}", lambda e: e.dma_start(out=xt[t % 2], in_=x_src[t * P:(t + 1) * P, :]), reads=[self.DB(xkey)], writes=[bx[t % 2]])
            ld(0)
            for t in range(NTILE):
                i = t % 2; blk = t // 4; j = t % 4
                if t + 1 < NTILE: ld(t + 1)
                fw.op("act", lambda e: e.activation(out=xb[i], in_=xt[i], func=AF.Copy), reads=[bx[i]], writes=[bxb[i]])
                pb, pbuf = bk[t % 2]
                pv = pb.bitcast(BF16)
                for kc in range(KC):
                    fw.op("pe", lambda e, kc=kc: e.transpose(out=pv[:, kc * P:(kc + 1) * P], in_=xb[i][:, kc * P:(kc + 1) * P], identity=c["ident_bf"]),
                          reads=[bxb[i], c["b"]], writes=[pbuf])
                T = xT[blk % 2]
                fw.op("dve", lambda e: e.tensor_copy(out=T[:, :, j * P:(j + 1) * P], in_=pv.rearrange("p (k t) -> p k t", k=KC)), reads=[pbuf], writes=[bT[blk % 2]])
                if j == 3:
                    fw.dma("sp", f"x0st{blk % 2}", lambda e: e.dma_start(out=self.XT[blk], in_=T), reads=[bT[blk % 2]], writes=[self.DB("XT")])
            fw.barrier()

    def zero_scratch(self):
        fw = self.fw; c = self.c
        zt = c["zt"]
        R = 1024
        i = 0
        for tgt, rows in ((self.XS, NSLOT), (self.YS, NSLOT)):
            for r0 in range(0, rows, R):
                fw.dma("sp", f"zf{i % 4}", lambda e: e.dma_start(out=tgt[r0:r0 + R, :].rearrange("(p j) d -> p j d", p=P), in_=zt), reads=[c["b"]])
                i += 1

    def pass_c(self, l):
        fw = self.fw; c = self.c; cb = c["b"]
        with ExitStack() as st:
            sb = lambda n, s, d: self.sb(st, n, s, d)
            bk = self.banks(st)
            Wc = sb("c_W", [P, KC, 3072], BF16); Wco = sb("c_Wco", [P, KC, D], BF16)
            bW = Buf("c_W")
            pc = c["pcol"]; bpc = c["bpcol"]
            self.load_w(Wc[:, :, 0:2048], self.g_win[l], 0, 2048, self.DB(f"g_win{l}"), bW, "wl0")
            self.load_w(Wc[:, :, 2048:3072], self.g_win[l], OFF_GA, OFF_GA + 1024, self.DB(f"g_win{l}"), bW, "wl1")
            self.load_w(Wco, self.g_sm[l][0], 0, D, self.DB(f"g_sm{l}_0"), bW, "wl2")
            if l == 0:
                self.gathers(0, ("w2",))
            if l + 1 < self.Lp:
                self.gathers(l + 1, ("win", "sm", "w2"))
            xT = [sb(f"c_xT{i}", [P, KC, TB], BF16) for i in range(2)]; bxT = [Buf("c_xT0"), Buf("c_xT1")]
            z = sb("c_z", [P, KC, TB + 30], BF16); bz = [Buf(f"c_z{i}") for i in range(KC)]
            dg = [sb(f"c_dg{i}", [P, TAPS, P], BF16) for i in range(2)]; bdg = [Buf("c_dg0"), Buf("c_dg1")]
            cv = sb("c_cv", [P, KC, TB], F32); bcv = [Buf(f"c_cv{i}") for i in range(KC)]
            sg = [sb(f"c_sg{i}", [P, TB], F32) for i in range(2)]; bsg = [Buf("c_sg0"), Buf("c_sg1")]
            zb = [sb(f"c_zb{i}", [P, TB], BF16) for i in range(2)]; bzb = [Buf("c_zb0"), Buf("c_zb1")]
            zs = [sb(f"c_zs{i}", [P, TB], BF16) for i in range(2)]; bzs = [Buf("c_zs0"), Buf("c_zs1")]
            mean = sb("c_mean", [P, TB], F32); m2 = sb("c_m2", [P, TB], F32); rstd = sb("c_rstd", [P, TB], F32); bst = Buf("c_stats")
            zn = sb("c_zn", [P, KC, TB], BF16); bzn = Buf("c_zn")
            gct = [sb(f"c_gct{i}", [P, KC, TB], BF16) for i in range(2)]; bgct = [Buf("c_gct0"), Buf("c_gct1")]
            col = lambda nm, i: pc[:, PCOL[nm] + i:PCOL[nm] + i + 1]
            S1, bS1 = bk[4]; S2, bS2 = bk[5]
            def ld(b):
                fw.dma("sp", f"c_ld{b % 2}", lambda e: e.dma_start(out=xT[b % 2], in_=self.XT[b]), reads=[self.DB("XT")], writes=[bxT[b % 2]])
            ld(0)
            for b in range(NBLK):
                i = b % 2
                if b + 1 < NBLK: ld(b + 1)
                if b % BPS == 0:
                    fw.op("dve", lambda e: e.memset(z[:, :, 0:30], 0.0), writes=bz)
                for ch in range(KC):
                    r = ch % 2
                    pa, bpa = bk[2 * r]; pg, bpg = bk[2 * r + 1]
                    for kc in range(KC):
                        fw.op("pe", lambda e, kc=kc: e.matmul(pa, lhsT=Wc[:, kc, ch * P:(ch + 1) * P], rhs=xT[i][:, kc, :], start=(kc == 0), stop=(kc == KC - 1)),
                              reads=[bW, bxT[i]], writes=[bpa])
                    for kc in range(KC):
                        fw.op("pe", lambda e, kc=kc: e.matmul(pg, lhsT=Wc[:, kc, D + ch * P:D + (ch + 1) * P], rhs=xT[i][:, kc, :], start=(kc == 0), stop=(kc == KC - 1)),
                              reads=[bW, bxT[i]], writes=[bpg])
                    fw.op("act", lambda e: e.activation(out=sg[r], in_=pg, func=AF.Sigmoid, bias=col("b_g", ch), scale=1.0), reads=[bpg, bpc], writes=[bsg[r]])
                    fw.op("dve", lambda e: e.scalar_tensor_tensor(out=z[:, ch, 30:30 + TB], in0=pa, scalar=col("b_a", ch), in1=sg[r], op0=ALU.add, op1=ALU.mult),
                          reads=[bpa, bsg[r], bpc], writes=[bz[ch]])
                    wcol = lambda j: pc[:, PCOL["dw_w"] + ch * TAPS + j:PCOL["dw_w"] + ch * TAPS + j + 1]
                    for j in range(TAPS):
                        fw.op("dve", lambda e, j=j: e.tensor_scalar(out=dg[r][:, j, :], in0=c["ident_bf"], scalar1=wcol(j), scalar2=None, op0=ALU.mult), reads=[cb, bpc], writes=[bdg[r]])
                    pcv, bpcv = bk[6 + r]
                    for j in range(TAPS):
                        fw.op("pe", lambda e, j=j: e.matmul(pcv, lhsT=dg[r][:, j, :], rhs=z[:, ch, j:j + TB], start=(j == 0), stop=(j == TAPS - 1)), reads=[bdg[r], bz[ch]], writes=[bpcv])
                    fw.op("act", lambda e: e.activation(out=z[:, ch, 0:30], in_=z[:, ch, TB:TB + 30], func=AF.Copy), reads=[bz[ch]], writes=[bz[ch]])
                    fw.op("act", lambda e: e.activation(out=cv[:, ch, :], in_=pcv, func=AF.Identity, bias=col("dw_b", ch), scale=1.0), reads=[bpcv, bpc], writes=[bcv[ch]])
                    fw.op("act", lambda e: e.activation(out=zb[r], in_=pcv, func=AF.Identity, bias=col("dw_b", ch), scale=1.0), reads=[bpcv, bpc], writes=[bzb[r]])
                    fw.op("act", lambda e: e.activation(out=zs[r], in_=pcv, func=AF.Square, bias=col("dw_b", ch), scale=1.0), reads=[bpcv, bpc], writes=[bzs[r]])
                    fw.op("pe", lambda e: e.matmul(S1, lhsT=c["ones_bf"], rhs=zb[r], start=(ch == 0), stop=(ch == KC - 1)), reads=[cb, bzb[r]], writes=[bS1])
                    fw.op("pe", lambda e: e.matmul(S2, lhsT=c["ones_bf"], rhs=zs[r], start=(ch == 0), stop=(ch == KC - 1)), reads=[cb, bzs[r]], writes=[bS2])
                fw.op("act", lambda e: e.activation(out=mean, in_=S1, func=AF.Identity, scale=1.0 / D), reads=[bS1], writes=[bst])
                fw.op("dve", lambda e: e.tensor_tensor(out=m2, in0=mean, in1=mean, op=ALU.mult), reads=[bst], writes=[bst])
                fw.op("dve", lambda e: e.scalar_tensor_tensor(out=m2, in0=S2, scalar=1.0 / D, in1=m2, op0=ALU.mult, op1=ALU.subtract), reads=[bS2, bst], writes=[bst])
                fw.op("act", lambda e: e.activation(out=m2, in_=m2, func=AF.Sqrt, bias=c["eps"], scale=1.0), reads=[bst], writes=[bst])
                fw.op("dve", lambda e: e.reciprocal(out=rstd, in_=m2), reads=[bst], writes=[bst])
                for ch in range(KC):
                    fw.op("dve", lambda e: e.tensor_tensor(out=cv[:, ch, :], in0=cv[:, ch, :], in1=mean, op=ALU.subtract), reads=[bcv[ch], bst], writes=[bcv[ch]])
                    fw.op("dve", lambda e: e.tensor_tensor(out=cv[:, ch, :], in0=cv[:, ch, :], in1=rstd, op=ALU.mult), reads=[bcv[ch], bst], writes=[bcv[ch]])
                    fw.op("act", lambda e: e.activation(out=zn[:, ch, :], in_=cv[:, ch, :], func=AF.Silu, bias=col("cn_b", ch), scale=col("cn_g", ch)),
                          reads=[bcv[ch], bpc], writes=[bzn])
                for d in range(KC):
                    r = d % 2
                    py, bpy = bk[2 * r]; pga, bpga = bk[2 * r + 1]
                    for ch in range(KC):
                        fw.op("pe", lambda e, ch=ch: e.matmul(py, lhsT=Wco[:, ch, d * P:(d + 1) * P], rhs=zn[:, ch, :], start=(ch == 0), stop=(ch == KC - 1)),
                              reads=[bW, bzn], writes=[bpy])
                    for kc in range(KC):
                        fw.op("pe", lambda e, kc=kc: e.matmul(pga, lhsT=Wc[:, kc, 2048 + d * P:2048 + (d + 1) * P], rhs=xT[i][:, kc, :], start=(kc == 0), stop=(kc == KC - 1)),
                              reads=[bW, bxT[i]], writes=[bpga])
                    fw.op("act", lambda e: e.activation(out=sg[r], in_=pga, func=AF.Sigmoid, bias=col("b_ga", d), scale=1.0), reads=[bpga, bpc], writes=[bsg[r]])
                    fw.op("dve", lambda e: e.scalar_tensor_tensor(out=gct[i][:, d, :], in0=py, scalar=col("co_b", d), in1=sg[r], op0=ALU.add, op1=ALU.mult),
                          reads=[bpy, bsg[r], bpc], writes=[bgct[i]])
                fw.dma("sp", f"c_st{i}", lambda e: e.dma_start(out=self.GCT[b], in_=gct[i]), reads=[bgct[i]], writes=[self.DB("GCT")])
            fw.barrier()

    def pass_m1(self, l):
        fw = self.fw; c = self.c; cb = c["b"]
        with ExitStack() as st:
            sb = lambda n, s, d: self.sb(st, n, s, d)
            bk = self.banks(st)
            NW = 4104
            Wm = sb("m_W", [P, KC, NW], BF16); bW = Buf("m_W")
            self.load_w(Wm, self.g_win[l], OFF_Q, OFF_Q + NW, self.DB(f"g_win{l}"), bW, "wl0")
            if l == 0:
                self.gathers(0, ("w1",))
            if l + 1 < self.Lp:
                self.gathers(l + 1, ("w1",))
            pc = c["pcol"]; bpc = c["bpcol"]; pr = c["prow_bf"]; bpr = c["bprow"]
            col = lambda nm, i: pc[:, PCOL[nm] + i:PCOL[nm] + i + 1]
            xT = [sb(f"m_xT{i}", [P, KC, TB], BF16) for i in range(2)]; bxT = [Buf("m_xT0"), Buf("m_xT1")]
            raw = [sb(f"m_raw{i}", [P, TB + 3], F32) for i in range(2)]; braw = [Buf("m_raw0"), Buf("m_raw1")]
            halo = sb("m_halo", [P, 16, 3], F32); bhalo = Buf("m_halo")
            acc = [sb(f"m_acc{i}", [P, TB], F32) for i in range(2)]; bacc = [Buf("m_acc0"), Buf("m_acc1")]
            qkT = sb("m_qkT", [P, 16, TB], BF16); bqk = Buf("m_qkT")
            soT = sb("m_soT", [P, KC, TB], BF16); bso = Buf("m_soT")
            hgt = [sb(f"m_hgt{i}", [P, KC, TB], BF16) for i in range(2)]; bhgt = [Buf("m_hgt0"), Buf("m_hgt1")]
            vp = [sb(f"m_vp{i}", [P, 4, 257], BF16) for i in range(2)]; bvp = [Buf("m_vp0"), Buf("m_vp1")]
            C = sb("m_C", [P, 4, 2, 257], F32); Cb = sb("m_Cb", [P, 4, 2, 257], BF16); bC = [Buf(f"m_C{h}") for h in range(4)]
            E = sb("m_E", [P, 512], F32); bE = Buf("m_E")
            lfb = sb("m_lfb", [P, 4, P], F32); blfb = Buf("m_lfb")
            ifs = sb("m_ifs", [P, 8], F32); ef = sb("m_ef", [P, 4], F32); lfn = sb("m_lfn", [P, 4], F32)
            arg = sb("m_arg", [P, 4], F32); wk = sb("m_wk", [P, 4], F32); bg = Buf("m_gates")
            qtl = [sb(f"m_qtl{h}", [P, 2, P], BF16) for h in range(4)]; bqtl = [Buf(f"m_qtl{h}") for h in range(4)]
            AT = [sb(f"m_AT{h}", [P, P], BF16) for h in range(4)]; bAT = [Buf(f"m_AT{h}") for h in range(4)]
            ktok = [sb(f"m_ktok{h}", [P, 256], BF16) for h in range(4)]; bktok = [Buf(f"m_ktok{h}") for h in range(4)]
            hh = [sb(f"m_hh{h}", [P, 256], F32) for h in range(4)]; bhh = [Buf(f"m_hh{h}") for h in range(4)]
            hn = [sb(f"m_hn{h}", [P, 256], BF16) for h in range(4)]; bhn = [Buf(f"m_hn{h}") for h in range(4)]
            sm = [sb(f"m_sm{h}", [P, 16], F32) for h in range(4)]; bsm = [Buf(f"m_sm{h}") for h in range(4)]
            stt = [sb(f"m_stt{i}", [P, 257], F32) for i in range(4)]; bstt = [Buf(f"m_stt{i}") for i in range(4)]
            for i in range(2):
                fw.op("dve", lambda e, i=i: e.memset(vp[i][:, :, 256:257], 1.0), writes=[bvp[i]])
            def ld(b):
                fw.dma("sp", f"m_ld{b % 2}", lambda e: e.dma_start(out=xT[b % 2], in_=self.XT[b]), reads=[self.DB("XT")], writes=[bxT[b % 2]])
            ld(0)
            for b in range(NBLK):
                i = b % 2
                if b + 1 < NBLK: ld(b + 1)
                if b % BPS == 0:
                    fw.op("dve", lambda e: e.memset(halo, 0.0), writes=[bhalo])
                    fw.op("dve", lambda e: e.memset(C, 0.0), writes=bC)
                    fw.op("dve", lambda e: e.memset(Cb, 0.0), writes=bC)
                for ch in range(16):
                    r = ch % 2
                    pq, bpq = bk[r]
                    for kc in range(KC):
                        fw.op("pe", lambda e, kc=kc: e.matmul(pq, lhsT=Wm[:, kc, ch * P:(ch + 1) * P], rhs=xT[i][:, kc, :], start=(kc == 0), stop=(kc == KC - 1)),
                              reads=[bW, bxT[i]], writes=[bpq])
                    bcol = col("b_q", ch) if ch < 8 else col("b_k", ch - 8)
                    fw.op("act", lambda e: e.activation(out=raw[r][:, 0:3], in_=halo[:, ch, :], func=AF.Copy), reads=[bhalo], writes=[braw[r]])
                    fw.op("act", lambda e: e.activation(out=raw[r][:, 3:3 + TB], in_=pq, func=AF.Identity, bias=bcol, scale=1.0), reads=[bpq, bpc], writes=[braw[r]])
                    fw.op("act", lambda e: e.activation(out=halo[:, ch, :], in_=raw[r][:, TB:TB + 3], func=AF.Copy), reads=[braw[r]], writes=[bhalo])
                    wq = lambda j: pc[:, PCOL["qk_w"] + ch * 4 + j:PCOL["qk_w"] + ch * 4 + j + 1]
                    fw.op("dve", lambda e: e.tensor_scalar(out=acc[r], in0=raw[r][:, 0:TB], scalar1=wq(0), scalar2=col("qk_b", ch), op0=ALU.mult, op1=ALU.add),
                          reads=[braw[r], bpc], writes=[bacc[r]])
                    for j in range(1, 4):
                        fw.op("dve", lambda e, j=j: e.scalar_tensor_tensor(out=acc[r], in0=raw[r][:, j:j + TB], scalar=wq(j), in1=acc[r], op0=ALU.mult, op1=ALU.add),
                              reads=[braw[r], bacc[r], bpc], writes=[bacc[r]])
                    fw.op("act", lambda e: e.activation(out=qkT[:, ch, :], in_=acc[r], func=AF.Silu), reads=[bacc[r]], writes=[bqk])
                for ch in range(KC):
                    r = ch % 2
                    po, bpo = bk[r]
                    for kc in range(KC):
                        fw.op("pe", lambda e, kc=kc: e.matmul(po, lhsT=Wm[:, kc, 3072 + ch * P:3072 + (ch + 1) * P], rhs=xT[i][:, kc, :], start=(kc == 0), stop=(kc == KC - 1)),
                              reads=[bW, bxT[i]], writes=[bpo])
                    fw.op("act", lambda e: e.activation(out=soT[:, ch, :], in_=po, func=AF.Sigmoid, bias=col("b_o", ch), scale=1.0), reads=[bpo, bpc], writes=[bso])
                for j4 in range(4):
                    t0 = j4 * P; vi = j4 % 2
                    xs = lambda kc: xT[i][:, kc, t0:t0 + P]
                    for hf in range(2):
                        pv, bpv = bk[hf]
                        for kc in range(KC):
                            fw.op("pe", lambda e, kc=kc: e.matmul(pv, lhsT=xs(kc), rhs=Wm[:, kc, 2048 + hf * 512:2048 + (hf + 1) * 512], start=(kc == 0), stop=False),
                                  reads=[bW, bxT[i]], writes=[bpv])
                        fw.op("pe", lambda e: e.matmul(pv, lhsT=c["ones_bf"][0:1, :], rhs=pr[0:1, PROW["b_v"] + hf * 512:PROW["b_v"] + (hf + 1) * 512], start=False, stop=True),
                              reads=[cb, bpr], writes=[bpv])
                        fw.op("act", lambda e: e.activation(out=vp[vi][:, 2 * hf:2 * hf + 2, 0:256], in_=pv.rearrange("p (h d) -> p h d", h=2), func=AF.Copy),
                              reads=[bpv], writes=[bvp[vi]])
                    pgt, bpgt = bk[2]
                    for kc in range(KC):
                        fw.op("pe", lambda e, kc=kc: e.matmul(pgt[:, 0:8], lhsT=xs(kc), rhs=Wm[:, kc, 4096:4104], start=(kc == 0), stop=False), reads=[bW, bxT[i]], writes=[bpgt])
                    fw.op("pe", lambda e: e.matmul(pgt[:, 0:8], lhsT=c["ones_bf"][0:1, :], rhs=pr[0:1, PROW["b_if"]:PROW["b_if"] + 8], start=False, stop=True),
                          reads=[cb, bpr], writes=[bpgt])
                    fw.op("act", lambda e: e.activation(out=ifs, in_=pgt[:, 0:8], func=AF.Copy), reads=[bpgt], writes=[bg])
                    fw.op("act", lambda e: e.activation(out=ef, in_=ifs[:, 4:8], func=AF.Exp, scale=-1.0), reads=[bg], writes=[bg])
                    fw.op("act", lambda e: e.activation(out=lfn, in_=ef, func=AF.Ln, bias=c["one"], scale=1.0), reads=[bg], writes=[bg])
                    fw.op("pe", lambda e: e.matmul(pgt[:, 8:12], lhsT=c["tri_incl"], rhs=lfn, start=True, stop=True), reads=[cb, bg], writes=[bpgt])
                    fw.op("dve", lambda e: e.tensor_tensor(out=arg, in0=ifs[:, 0:4], in1=pgt[:, 8:12], op=ALU.add), reads=[bg, bpgt], writes=[bg])
                    fw.op("act", lambda e: e.activation(out=wk, in_=arg, func=AF.Exp, bias=c["nln16"], scale=1.0), reads=[bg], writes=[bg])
                    for h in range(4):
                        fw.op("dve", lambda e, h=h: e.tensor_scalar(out=lfb[:, h, :], in0=c["ones_f"], scalar1=lfn[:, h:h + 1], scalar2=None, op0=ALU.mult), reads=[bg, cb], writes=[blfb])
                    pbt, bpbt = bk[3]
                    for h in range(4):
                        fw.op("pe", lambda e, h=h: e.matmul(pbt[:, h * P:(h + 1) * P], lhsT=lfb[:, h, :], rhs=c["tri_incl"], start=True, stop=True), reads=[blfb, cb], writes=[bpbt])
                    fw.op("act", lambda e: e.activation(out=E, in_=pbt, func=AF.Exp, scale=-1.0), reads=[bpbt], writes=[bE])
                    def bset(h):
                        return (bk[4], bk[5], bk[6], bk[7]) if h % 2 == 0 else (bk[0], bk[1], bk[2], bk[3])
                    for h in range(4):
                        (pat, bpat), (ptr, bptr), _, _ = bset(h); ptrb = ptr.bitcast(BF16)
                        for dc in range(2):
                            fw.op("dve", lambda e, dc=dc: e.tensor_tensor(out=qtl[h][:, dc, :], in0=qkT[:, 2 * h + dc, t0:t0 + P], in1=E[:, h * P:(h + 1) * P], op=ALU.mult),
                                  reads=[bqk, bE], writes=[bqtl[h]])
                        for dc in range(2):
                            fw.op("pe", lambda e, dc=dc: e.matmul(pat[:, 0:P], lhsT=qkT[:, 8 + 2 * h + dc, t0:t0 + P], rhs=qtl[h][:, dc, :], start=(dc == 0), stop=(dc == 1)),
                                  reads=[bqk, bqtl[h]], writes=[bpat])
                        for dc in range(2):
                            fw.op("pe", lambda e, dc=dc: e.transpose(out=ptrb[:, dc * P:(dc + 1) * P], in_=qkT[:, 8 + 2 * h + dc, t0:t0 + P], identity=c["ident_bf"]),
                                  reads=[bqk, cb], writes=[bptr])
                        fw.op("dve", lambda e: e.scalar_tensor_tensor(out=AT[h], in0=pat[:, 0:P], scalar=wk[:, h:h + 1], in1=c["tri_incl"], op0=ALU.mult, op1=ALU.mult),
                              reads=[bpat, bg, cb], writes=[bAT[h]])
                        fw.op("act", lambda e: e.activation(out=ktok[h], in_=ptrb[:, 0:256], func=AF.Identity, scale=wk[:, h:h + 1]), reads=[bptr, bg], writes=[bktok[h]])
                    for h in range(4):
                        (pat, bpat), (ptr, bptr), (pn, bpn), (pst0, bpst0) = bset(h)
                        eL = E[:, h * P + P - 1:h * P + P]
                        fw.op("pe", lambda e: e.matmul(pn[:, 0:257], lhsT=AT[h], rhs=vp[vi][:, h, :], start=True, stop=False), reads=[bAT[h], bvp[vi]], writes=[bpn])
                        for dc in range(2):
                            fw.op("pe", lambda e, dc=dc: e.matmul(pn[:, 0:257], lhsT=qtl[h][:, dc, :], rhs=Cb[:, h, dc, :], start=False, stop=(dc == 1)), reads=[bqtl[h], bC[h]], writes=[bpn])
                        for dc in range(2):
                            pst, bpst = (pst0, bpst0) if dc == 0 else (pat, bpat)
                            o0 = 0 if dc == 0 else 128
                            fw.op("pe", lambda e, dc=dc: e.matmul(pst[:, o0:o0 + 257], lhsT=ktok[h][:, dc * P:(dc + 1) * P], rhs=vp[vi][:, h, :], start=True, stop=True),
                                  reads=[bktok[h], bvp[vi]], writes=[bpst])
                        smh = sm[h]; bsmh = bsm[h]
                        fw.op("act", lambda e: e.activation(out=smh[:, 0:1], in_=pn[:, 256:257], func=AF.Abs), reads=[bpn], writes=[bsmh])
                        fw.op("dve", lambda e: e.tensor_scalar(out=smh[:, 0:1], in0=smh[:, 0:1], scalar1=1.0, scalar2=None, op0=ALU.max), reads=[bsmh], writes=[bsmh])
                        fw.op("dve", lambda e: e.reciprocal(out=smh[:, 1:2], in_=smh[:, 0:1]), reads=[bsmh], writes=[bsmh])
                        fw.op("act", lambda e: e.activation(out=hh[h], in_=pn[:, 0:256], func=AF.Identity, scale=smh[:, 1:2]), reads=[bpn, bsmh], writes=[bhh[h]])
                        fw.op("dve", lambda e: e.bn_stats(out=smh[:, 2:8], in_=hh[h]), reads=[bhh[h]], writes=[bsmh])
                        fw.op("dve", lambda e: e.bn_aggr(out=smh[:, 8:10], in_=smh[:, 2:8]), reads=[bsmh], writes=[bsmh])
                        fw.op("act", lambda e: e.activation(out=smh[:, 10:11], in_=smh[:, 9:10], func=AF.Sqrt, bias=c["eps"], scale=1.0), reads=[bsmh], writes=[bsmh])
                        fw.op("dve", lambda e: e.reciprocal(out=smh[:, 11:12], in_=smh[:, 10:11]), reads=[bsmh], writes=[bsmh])
                        fw.op("dve", lambda e: e.tensor_scalar(out=hn[h], in0=hh[h], scalar1=smh[:, 8:9], scalar2=smh[:, 11:12], op0=ALU.subtract, op1=ALU.mult), reads=[bhh[h], bsmh], writes=[bhn[h]])
                        for dc in range(2):
                            pst, bpst = (pst0, bpst0) if dc == 0 else (pat, bpat)
                            o0 = 0 if dc == 0 else 128
                            sti = (2 * h + dc) % 4
                            fw.op("act", lambda e, dc=dc: e.activation(out=stt[sti], in_=pst[:, o0:o0 + 257], func=AF.Identity, scale=eL), reads=[bpst, bE], writes=[bstt[sti]])
                            fw.op("dve", lambda e, dc=dc: e.scalar_tensor_tensor(out=C[:, h, dc, :], in0=C[:, h, dc, :], scalar=eL, in1=stt[sti], op0=ALU.mult, op1=ALU.add),
                                  reads=[bstt[sti], bC[h], bE], writes=[bC[h]])
                            fw.op("act", lambda e, dc=dc: e.activation(out=Cb[:, h, dc, :], in_=C[:, h, dc, :], func=AF.Copy), reads=[bC[h]], writes=[bC[h]])
                    for h in range(4):
                        _, (ptr, bptr), _, _ = bset(h); ptrb = ptr.bitcast(BF16)
                        for dc in range(2):
                            fw.op("pe", lambda e, dc=dc: e.transpose(out=ptrb[:, 256 + dc * P:256 + (dc + 1) * P], in_=hn[h][:, dc * P:(dc + 1) * P], identity=c["ident_bf"]),
                                  reads=[bhn[h], cb], writes=[bptr])
                        for dc in range(2):
                            fw.op("dve", lambda e, dc=dc: e.scalar_tensor_tensor(out=hgt[i][:, 2 * h + dc, t0:t0 + P], in0=ptrb[:, 256 + dc * P:256 + (dc + 1) * P],
                                                                                   scalar=col("mn_g", 2 * h + dc), in1=soT[:, 2 * h + dc, t0:t0 + P], op0=ALU.mult, op1=ALU.mult),
                                  reads=[bptr, bpc, bso], writes=[bhgt[i]])
                fw.dma("sp", f"m_st{i}", lambda e: e.dma_start(out=self.HGT[b], in_=hgt[i]), reads=[bhgt[i]], writes=[self.DB("HGT")])
            fw.barrier()

    def pass_m2(self, l, x_src, xkey):
        fw = self.fw; c = self.c; cb = c["b"]
        with ExitStack() as st:
            sb = lambda n, s, d: self.sb(st, n, s, d)
            bk = self.banks(st)
            Wgb = sb("n_Wgb", [P, KC, D], BF16); Wmo = sb("n_Wmo", [P, KC, D], BF16); Wwo = sb("n_Wwo", [P, KC, D], BF16); bW = Buf("n_W")
            self.load_w(Wgb, self.g_win[l], OFF_GB, OFF_GB + D, self.DB(f"g_win{l}"), bW, "wl0")
            self.load_w(Wmo, self.g_sm[l][1], 0, D, self.DB(f"g_sm{l}_1"), bW, "wl1")
            self.load_w(Wwo, self.g_sm[l][2], 0, D, self.DB(f"g_sm{l}_2"), bW, "wl2")
            rw = sb("n_rw", [P, KC, NE], F32); brw = Buf("n_rw")
            fw.dma("sp", "n_rw", lambda e: e.dma_start(out=rw, in_=self.rw[l].rearrange("(kc p) e -> p kc e", p=P)), writes=[brw])
            G1 = sb("n_G1", [P, D], F32); B1 = sb("n_B1", [P, D], F32); bGB = Buf("n_GB")
            fw.dma("sp", "n_g1", lambda e: e.dma_start(out=G1, in_=self.pbc[l, 0, :].partition_broadcast(P)), writes=[bGB])
            fw.dma("sp", "n_b1", lambda e: e.dma_start(out=B1, in_=self.pbc[l, 1, :].partition_broadcast(P)), writes=[bGB])
            pc = c["pcol"]; bpc = c["bpcol"]; prf = c["prow_f"]; bpr = c["bprow"]
            col = lambda nm, i: pc[:, PCOL[nm] + i:PCOL[nm] + i + 1]
            xT = [sb(f"n_xT{i}", [P, KC, TB], BF16) for i in range(2)]; bxT = [Buf("n_xT0"), Buf("n_xT1")]
            hg = [sb(f"n_hg{i}", [P, KC, TB], BF16) for i in range(2)]; bhg = [Buf("n_hg0"), Buf("n_hg1")]
            gc = [sb(f"n_gc{i}", [P, KC, TB], BF16) for i in range(2)]; bgc = [Buf("n_gc0"), Buf("n_gc1")]
            sgb = [sb(f"n_sgb{i}", [P, TB], F32) for i in range(2)]; bsgb = [Buf("n_sgb0"), Buf("n_sgb1")]
            mix = sb("n_mix", [P, KC, TB], BF16); bmix = Buf("n_mix")
            xt = [sb(f"n_xt{i}", [P, D], F32) for i in range(2)]; bxt = [Buf("n_xt0"), Buf("n_xt1")]
            x1 = [sb(f"n_x1{i}", [P, D], F32) for i in range(2)]; bx1 = [Buf("n_x10"), Buf("n_x11")]
            x1b = [sb(f"n_x1b{i}", [P, D], BF16) for i in range(2)]; bx1b = [Buf("n_x1b0"), Buf("n_x1b1")]
            x1T = sb("n_x1T", [P, KC, P], F32); bx1T = Buf("n_x1T")
            st6 = sb("n_st6", [P, 2, 6], F32); mv = sb("n_mv", [P, 2], F32); sd = sb("n_sd", [P, 1], F32); rs = sb("n_rs", [P, 1], F32); bln = Buf("n_ln")
            lg = sb("n_lg", [P, NE], F32); v8 = sb("n_v8", [P, 8], F32); i8 = sb("n_i8", [P, 8], U32); i8f = sb("n_i8f", [P, 8], F32)
            e4 = sb("n_e4", [P, 4], F32); sm = sb("n_sm", [P, 8], F32); M = sb("n_M", [P, NE], F32); RC = sb("n_RC", [P, NE], F32); ovf = sb("n_ovf", [P, NE], F32)
            scr = sb("n_scr", [P, NE], F32); destf = sb("n_destf", [P, 4], F32); gm = sb("n_gm", [P, 4], F32); brt = Buf("n_rt")
            cnt = sb("n_cnt", [1, NE], F32); bcnt = Buf("n_cnt")
            fw.op("pool", lambda e: e.memset(cnt, 0.0), writes=[bcnt])
            dest = c["dest"]; gate = c["gate"]; bdest = c["bdest"]
            def ldb(b):
                i = b % 2
                fw.dma("sp", f"n_ldx{i}", lambda e: e.dma_start(out=xT[i], in_=self.XT[b]), reads=[self.DB("XT")], writes=[bxT[i]])
                fw.dma("sp", f"n_ldh{i}", lambda e: e.dma_start(out=hg[i], in_=self.HGT[b]), reads=[self.DB("HGT")], writes=[bhg[i]])
                fw.dma("sp", f"n_ldg{i}", lambda e: e.dma_start(out=gc[i], in_=self.GCT[b]), reads=[self.DB("GCT")], writes=[bgc[i]])
            def ldt(t):
                fw.dma("sp", f"n_ldt{t % 2}", lambda e: e.dma_start(out=xt[t % 2], in_=x_src[t * P:(t + 1) * P, :]), reads=[self.DB(xkey)], writes=[bxt[t % 2]])
            ldb(0); ldt(0)
            for b in range(NBLK):
                i = b % 2
                if b + 1 < NBLK: ldb(b + 1)
                for d in range(KC):
                    r = d % 2
                    py, bpy = bk[2 * r]; pgb, bpgb = bk[2 * r + 1]
                    for ch in range(KC):
                        fw.op("pe", lambda e, ch=ch: e.matmul(py, lhsT=Wmo[:, ch, d * P:(d + 1) * P], rhs=hg[i][:, ch, :], start=(ch == 0), stop=(ch == KC - 1)), reads=[bW, bhg[i]], writes=[bpy])
                    for kc in range(KC):
                        fw.op("pe", lambda e, kc=kc: e.matmul(pgb, lhsT=Wgb[:, kc, d * P:(d + 1) * P], rhs=xT[i][:, kc, :], start=(kc == 0), stop=(kc == KC - 1)), reads=[bW, bxT[i]], writes=[bpgb])
                    fw.op("act", lambda e: e.activation(out=sgb[r], in_=pgb, func=AF.Sigmoid, bias=col("b_gb", d), scale=1.0), reads=[bpgb, bpc], writes=[bsgb[r]])
                    fw.op("dve", lambda e: e.tensor_tensor(out=sgb[r], in0=py, in1=sgb[r], op=ALU.mult), reads=[bpy, bsgb[r]], writes=[bsgb[r]])
                    fw.op("dve", lambda e: e.tensor_tensor(out=mix[:, d, :], in0=sgb[r], in1=gc[i][:, d, :], op=ALU.add), reads=[bsgb[r], bgc[i]], writes=[bmix])
                for j4 in range(4):
                    t = b * 4 + j4; t0 = j4 * P; ti = t % 2
                    if t + 1 < NTILE: ldt(t + 1)
                    for hf in range(2):
                        po, bpo = bk[4 + hf]
                        for d in range(KC):
                            fw.op("pe", lambda e, d=d: e.matmul(po, lhsT=mix[:, d, t0:t0 + P], rhs=Wwo[:, d, hf * 512:(hf + 1) * 512], start=(d == 0), stop=(d == KC - 1)), reads=[bW, bmix], writes=[bpo])
                        fw.op("dve", lambda e, hf=hf: e.scalar_tensor_tensor(out=xt[ti][:, hf * 512:(hf + 1) * 512], in0=xt[ti][:, hf * 512:(hf + 1) * 512], scalar=ALPHA, in1=po, op0=ALU.mult, op1=ALU.add),
                              reads=[bxt[ti], bpo], writes=[bxt[ti]])
                    self.ln_tok(xt[ti], x1[ti], G1, B1, st6, mv, sd, rs, [bxt[ti]], [bln, bx1[ti]], bGB)
                    fw.dma("sp", f"n_stx{ti}", lambda e: e.dma_start(out=self.X1[t * P:(t + 1) * P, :], in_=x1[ti]), reads=[bx1[ti]], writes=[self.DB("X1")])
                    fw.op("act", lambda e: e.activation(out=x1b[ti], in_=x1[ti], func=AF.Copy), reads=[bx1[ti]], writes=[bx1b[ti]])
                    for hf in range(2):
                        ptr, bptr = bk[6 + hf]
                        for q in range(4):
                            kc = hf * 4 + q
                            fw.op("pe", lambda e, kc=kc, q=q: e.transpose(out=ptr[:, q * P:(q + 1) * P], in_=x1[ti][:, kc * P:(kc + 1) * P], identity=c["ident_f"]), reads=[bx1[ti], cb], writes=[bptr])
                        fw.op("act", lambda e, hf=hf: e.activation(out=x1T[:, hf * 4:hf * 4 + 4, :], in_=ptr.rearrange("p (k t) -> p k t", k=4), func=AF.Copy), reads=[bptr], writes=[bx1T])
                    plg, bplg = bk[0]
                    for kc in range(KC):
                        fw.op("pe", lambda e, kc=kc: e.matmul(plg[:, 0:NE], lhsT=x1T[:, kc, :], rhs=rw[:, kc, :], start=(kc == 0), stop=False), reads=[bx1T, brw], writes=[bplg])
                    fw.op("pe", lambda e: e.matmul(plg[:, 0:NE], lhsT=c["ones_f"][0:1, :], rhs=prf[0:1, 0:NE], start=False, stop=True), reads=[cb, bpr], writes=[bplg])
                    fw.op("act", lambda e: e.activation(out=lg, in_=plg[:, 0:NE], func=AF.Copy), reads=[bplg], writes=[brt])
                    fw.op("dve", lambda e: e.max(out=v8, in_=lg), reads=[brt], writes=[brt])
                    fw.op("dve", lambda e: e.max_index(out=i8, in_max=v8, in_values=lg), reads=[brt], writes=[brt])
                    fw.op("dve", lambda e: e.tensor_copy(out=i8f, in_=i8), reads=[brt], writes=[brt])
                    fw.op("dve", lambda e: e.tensor_scalar(out=sm[:, 0:1], in0=v8[:, 0:1], scalar1=-1.0, scalar2=None, op0=ALU.mult), reads=[brt], writes=[brt])
                    fw.op("act", lambda e: e.activation(out=e4, in_=v8[:, 0:4], func=AF.Exp, bias=sm[:, 0:1], scale=1.0), reads=[brt], writes=[brt])
                    fw.op("dve", lambda e: e.tensor_reduce(out=sm[:, 1:2], in_=e4, axis=mybir.AxisListType.X, op=ALU.add), reads=[brt], writes=[brt])
                    fw.op("dve", lambda e: e.reciprocal(out=sm[:, 2:3], in_=sm[:, 1:2]), reads=[brt], writes=[brt])
                    fw.op("dve", lambda e: e.tensor_scalar(out=M, in0=lg, scalar1=v8[:, 3:4], scalar2=None, op0=ALU.is_ge), reads=[brt], writes=[brt])
                    pR, bpR = bk[1]
                    fw.op("pe", lambda e: e.matmul(pR[:, 0:NE], lhsT=c["tri_strict"], rhs=M, start=True, stop=False), reads=[cb, brt], writes=[bpR])
                    fw.op("pe", lambda e: e.matmul(pR[:, 0:NE], lhsT=c["ones_f"][0:1, :], rhs=cnt, start=False, stop=True), reads=[cb, bcnt], writes=[bpR])
                    fw.op("dve", lambda e: e.tensor_scalar(out=ovf, in0=pR[:, 0:NE], scalar1=float(CAP), scalar2=1.0e7, op0=ALU.is_ge, op1=ALU.mult), reads=[bpR], writes=[brt])
                    fw.op("dve", lambda e: e.tensor_tensor(out=RC, in0=pR[:, 0:NE], in1=c["ecap"], op=ALU.add), reads=[bpR, cb], writes=[brt])
                    fw.op("dve", lambda e: e.tensor_tensor(out=RC, in0=RC, in1=ovf, op=ALU.add), reads=[brt], writes=[brt])
                    for k in range(TOPK):
                        fw.op("dve", lambda e, k=k: e.scalar_tensor_tensor(out=scr, in0=c["iota_e"], scalar=i8f[:, k:k + 1], in1=RC, op0=ALU.is_equal, op1=ALU.mult),
                              reads=[brt, cb], writes=[brt])
                        fw.op("dve", lambda e, k=k: e.tensor_reduce(out=destf[:, k:k + 1], in_=scr, axis=mybir.AxisListType.X, op=ALU.add), reads=[brt], writes=[brt])
                    fw.op("dve", lambda e: e.tensor_scalar(out=gm, in0=destf, scalar1=float(NSLOT), scalar2=None, op0=ALU.is_lt), reads=[brt], writes=[brt])
                    fw.op("dve", lambda e: e.tensor_scalar(out=destf, in0=destf, scalar1=float(NSLOT), scalar2=None, op0=ALU.min), reads=[brt], writes=[brt])
                    fw.op("dve", lambda e: e.tensor_copy(out=dest[:, t * 4:t * 4 + 4], in_=destf), reads=[brt], writes=[bdest])
                    fw.op("dve", lambda e: e.tensor_scalar(out=e4, in0=e4, scalar1=sm[:, 2:3], scalar2=None, op0=ALU.mult), reads=[brt], writes=[brt])
                    fw.op("dve", lambda e: e.tensor_tensor(out=gate[:, t * 4:t * 4 + 4], in0=e4, in1=gm, op=ALU.mult), reads=[brt], writes=[bdest])
                    pcs, bpcs = bk[2]
                    fw.op("pe", lambda e: e.matmul(pcs[0:1, 0:NE], lhsT=c["ones_f"][:, 0:1], rhs=M, start=True, stop=True), reads=[cb, brt], writes=[bpcs])
                    fw.op("dve", lambda e: e.tensor_tensor(out=cnt, in0=cnt, in1=pcs[0:1, 0:NE], op=ALU.add), reads=[bcnt, bpcs], writes=[bcnt])
                    for k in range(TOPK):
                        fw.dma("pool", f"n_sc{ti}{k}", lambda e, k=k: e.indirect_dma_start(out=self.XS, out_offset=bass.IndirectOffsetOnAxis(ap=dest[:, t * 4 + k:t * 4 + k + 1], axis=0),
                                                                                          in_=x1b[ti], in_offset=None, bounds_check=c["r_sc"], oob_is_err=False),
                               reads=[bx1b[ti], bdest])
            if self.debug:
                fw.dma("sp", "dbg0", lambda e: e.dma_start(out=self.dbg_dest, in_=dest), reads=[bdest])
                fw.dma("sp", "dbg1", lambda e: e.dma_start(out=self.dbg_gate, in_=gate), reads=[bdest])
                fw.dma("sp", "dbg2", lambda e: e.dma_start(out=self.dbg_cnt, in_=cnt), reads=[bcnt])
            fw.barrier()

    def pass_e(self, l):
        fw = self.fw; c = self.c; cb = c["b"]
        with ExitStack() as st:
            sb = lambda n, s, d: self.sb(st, n, s, d)
            bk = self.banks(st)
            W1 = [sb(f"e_W1{i}", [P, KC, 2 * DFF], BF16) for i in range(2)]
            W2 = [sb(f"e_W2{i}", [P, KC, D], BF16) for i in range(2)]
            b2r = [sb(f"e_b2{i}", [P, D], F32) for i in range(2)]; bb2 = [Buf("e_b2_0"), Buf("e_b2_1")]
            bW = [Buf("e_W0"), Buf("e_W1")]
            pc = c["pcol"]; bpc = c["bpcol"]
            xs = [sb(f"e_xs{i}", [P, 4, D], BF16) for i in range(2)]; bxs = [Buf("e_xs0"), Buf("e_xs1")]
            XTt = sb("e_XT", [P, KC, TB], BF16); bXT = Buf("e_XT")
            gcl = [sb(f"e_gc{i}", [P, TB], F32) for i in range(2)]; sgl = [sb(f"e_sg{i}", [P, TB], F32) for i in range(2)]; lcl = [sb(f"e_lc{i}", [P, TB], F32) for i in range(2)]
            bgl = [Buf("e_g0"), Buf("e_g1")]
            actT = sb("e_act", [P, KC, TB], BF16); bact = Buf("e_act")
            ys = [sb(f"e_ys{i}", [P, D], BF16) for i in range(2)]; bys = [Buf("e_ys0"), Buf("e_ys1")]

            def loadw(e_):
                i = e_ % 2
                src1 = self.g_w1[l][e_ * D:(e_ + 1) * D, :]
                src2 = self.g_w2[l][e_ * DFF:(e_ + 1) * DFF, :]
                self.load_w(W1[i], src1, 0, 2 * DFF, self.DB(f"g_w1{l}"), bW[i], f"e_w1{i}")
                self.load_w(W2[i], src2, 0, D, self.DB(f"g_w2{l}"), bW[i], f"e_w2{i}")
                fw.dma("sp", f"e_b2{i}", lambda e: e.dma_start(out=b2r[i], in_=self.prow[l, 0, PROW["b2"] + e_ * D:PROW["b2"] + (e_ + 1) * D].partition_broadcast(P)), writes=[bb2[i]])

            loadw(0)
            blist = [(e_, boff, bw) for e_ in range(NE) for (boff, bw) in EBLK]
            def ldx(n):
                e2, bo2, bw2 = blist[n]; s2 = e2 * CAP + bo2
                fw.dma("sp", f"e_ld{n % 2}", lambda e: e.dma_start(out=xs[n % 2][:, 0:bw2 // P, :], in_=self.XS[s2:s2 + bw2, :].rearrange("(j p) d -> p j d", p=P)), writes=[bxs[n % 2]])
            ldx(0)
            nb = 0
            for e_ in range(NE):
                wi = e_ % 2
                if e_ + 1 < NE:
                    loadw(e_ + 1)
                for (boff, bw) in EBLK:
                    s0 = e_ * CAP + boff; xi = nb % 2; nb += 1; nj = bw // P
                    if nb < len(blist): ldx(nb)
                    for j in range(nj):
                        ptr, bptr = bk[6 + (j % 2)]; pv = ptr.bitcast(BF16)
                        for kc in range(KC):
                            fw.op("pe", lambda e, kc=kc: e.transpose(out=pv[:, kc * P:(kc + 1) * P], in_=xs[xi][:, j, kc * P:(kc + 1) * P], identity=c["ident_bf"]), reads=[bxs[xi], cb], writes=[bptr])
                        fw.op("act", lambda e: e.activation(out=XTt[:, :, j * P:(j + 1) * P], in_=pv.rearrange("p (k t) -> p k t", k=KC), func=AF.Copy), reads=[bptr], writes=[bXT])
                    for ch in range(KC):
                        r = ch % 2
                        pg, bpg = bk[2 * r]; pl, bpl = bk[2 * r + 1]
                        for kc in range(KC):
                            fw.op("pe", lambda e, kc=kc: e.matmul(pg[:, 0:bw], lhsT=W1[wi][:, kc, ch * P:(ch + 1) * P], rhs=XTt[:, kc, 0:bw], start=(kc == 0), stop=(kc == KC - 1)), reads=[bW[wi], bXT], writes=[bpg])
                        for kc in range(KC):
                            fw.op("pe", lambda e, kc=kc: e.matmul(pl[:, 0:bw], lhsT=W1[wi][:, kc, DFF + ch * P:DFF + (ch + 1) * P], rhs=XTt[:, kc, 0:bw], start=(kc == 0), stop=(kc == KC - 1)), reads=[bW[wi], bXT], writes=[bpl])
                        b1g = pc[:, PCOL["b1"] + e_ * 16 + ch:PCOL["b1"] + e_ * 16 + ch + 1]
                        b1l = pc[:, PCOL["b1"] + e_ * 16 + 8 + ch:PCOL["b1"] + e_ * 16 + 8 + ch + 1]
                        fw.op("dve", lambda e: e.tensor_scalar(out=gcl[r][:, 0:bw], in0=pg[:, 0:bw], scalar1=b1g, scalar2=LIMIT, op0=ALU.add, op1=ALU.min), reads=[bpg, bpc], writes=[bgl[r]])
                        fw.op("act", lambda e: e.activation(out=sgl[r][:, 0:bw], in_=gcl[r][:, 0:bw], func=AF.Sigmoid, scale=SW_ALPHA), reads=[bgl[r]], writes=[bgl[r]])
                        fw.op("dve", lambda e: e.tensor_scalar(out=lcl[r][:, 0:bw], in0=pl[:, 0:bw], scalar1=b1l, scalar2=LIMIT, op0=ALU.add, op1=ALU.min), reads=[bpl, bpc], writes=[bgl[r]])
                        fw.op("dve", lambda e: e.tensor_scalar(out=lcl[r][:, 0:bw], in0=lcl[r][:, 0:bw], scalar1=-LIMIT, scalar2=1.0, op0=ALU.max, op1=ALU.add), reads=[bgl[r]], writes=[bgl[r]])
                        fw.op("pool", lambda e: e.tensor_tensor(out=gcl[r][:, 0:bw], in0=gcl[r][:, 0:bw], in1=sgl[r][:, 0:bw], op=ALU.mult), reads=[bgl[r]], writes=[bgl[r]])
                        fw.op("pool", lambda e: e.tensor_tensor(out=actT[:, ch, 0:bw], in0=gcl[r][:, 0:bw], in1=lcl[r][:, 0:bw], op=ALU.mult), reads=[bgl[r]], writes=[bact])
                    for j in range(nj):
                        yi = j % 2
                        for hf in range(2):
                            py, bpy = bk[4 + hf]
                            for ch in range(KC):
                                fw.op("pe", lambda e, ch=ch: e.matmul(py, lhsT=actT[:, ch, j * P:(j + 1) * P], rhs=W2[wi][:, ch, hf * 512:(hf + 1) * 512], start=(ch == 0), stop=(ch == KC - 1)), reads=[bact, bW[wi]], writes=[bpy])
                            fw.op("dve", lambda e, hf=hf: e.tensor_tensor(out=ys[yi][:, hf * 512:(hf + 1) * 512], in0=py, in1=b2r[wi][:, hf * 512:(hf + 1) * 512], op=ALU.add), reads=[bpy, bb2[wi]], writes=[bys[yi]])
                        fw.dma("sp", f"e_st{yi}", lambda e: e.dma_start(out=self.YS[s0 + j * P:s0 + (j + 1) * P, :], in_=ys[yi]), reads=[bys[yi]])
            fw.barrier()

    def pass_f(self, l, dst, dkey, make_xt):
        fw = self.fw; c = self.c; cb = c["b"]
        with ExitStack() as st:
            sb = lambda n, s, d: self.sb(st, n, s, d)
            bk = self.banks(st)
            G2 = sb("f_G2", [P, D], F32); B2 = sb("f_B2", [P, D], F32); bGB = Buf("f_GB")
            fw.dma("sp", "f_g2", lambda e: e.dma_start(out=G2, in_=self.pbc[l, 2, :].partition_broadcast(P)), writes=[bGB])
            fw.dma("sp", "f_b2", lambda e: e.dma_start(out=B2, in_=self.pbc[l, 3, :].partition_broadcast(P)), writes=[bGB])
            yg = [sb(f"f_yg{i}", [P, 4, D], BF16) for i in range(2)]; byg = [Buf("f_yg0"), Buf("f_yg1")]
            xt = [sb(f"f_xt{i}", [P, D], F32) for i in range(2)]; bxt = [Buf("f_xt0"), Buf("f_xt1")]
            acc = sb("f_acc", [P, D], F32); bacc = Buf("f_acc")
            xo = [sb(f"f_xo{i}", [P, D], F32) for i in range(2)]; bxo = [Buf("f_xo0"), Buf("f_xo1")]
            xb = [sb(f"f_xb{i}", [P, D], BF16) for i in range(2)]; bxb = [Buf("f_xb0"), Buf("f_xb1")]
            xT = [sb(f"f_xT{i}", [P, KC, TB], BF16) for i in range(2)]; bT = [Buf("f_T0"), Buf("f_T1")]
            st6 = sb("f_st6", [P, 2, 6], F32); mv = sb("f_mv", [P, 2], F32); sd = sb("f_sd", [P, 1], F32); rs = sb("f_rs", [P, 1], F32); bln = Buf("f_ln")
            dest = c["dest"]; gate = c["gate"]; bdest = c["bdest"]
            def ldf(t):
                i = t % 2
                for k in range(TOPK):
                    fw.dma("pool", f"f_g{i}{k}", lambda e, k=k: e.indirect_dma_start(out=yg[i][:, k, :], out_offset=None, in_=self.YS,
                                                                                     in_offset=bass.IndirectOffsetOnAxis(ap=dest[:, t * 4 + k:t * 4 + k + 1], axis=0), bounds_check=c["r_ga"], oob_is_err=False),
                           reads=[bdest], writes=[byg[i]])
                fw.dma("sp", f"f_ld{i}", lambda e: e.dma_start(out=xt[i], in_=self.X1[t * P:(t + 1) * P, :]), reads=[self.DB("X1")], writes=[bxt[i]])
            ldf(0)
            for t in range(NTILE):
                i = t % 2; blk = t // 4; j = t % 4
                if t + 1 < NTILE: ldf(t + 1)
                fw.op("dve", lambda e: e.tensor_scalar(out=acc, in0=yg[i][:, 0, :], scalar1=gate[:, t * 4:t * 4 + 1], scalar2=None, op0=ALU.mult), reads=[byg[i], bdest], writes=[bacc])
                for k in range(1, TOPK):
                    fw.op("dve", lambda e, k=k: e.scalar_tensor_tensor(out=acc, in0=yg[i][:, k, :], scalar=gate[:, t * 4 + k:t * 4 + k + 1], in1=acc, op0=ALU.mult, op1=ALU.add), reads=[byg[i], bdest, bacc], writes=[bacc])
                fw.op("dve", lambda e: e.scalar_tensor_tensor(out=xt[i], in0=xt[i], scalar=ALPHA, in1=acc, op0=ALU.mult, op1=ALU.add), reads=[bxt[i], bacc], writes=[bxt[i]])
                self.ln_tok(xt[i], xo[i], G2, B2, st6, mv, sd, rs, [bxt[i]], [bln, bxo[i]], bGB)
                fw.dma("sp", f"f_st{i}", lambda e: e.dma_start(out=dst[t * P:(t + 1) * P, :], in_=xo[i]), reads=[bxo[i]], writes=[self.DB(dkey)])
                if make_xt:
                    fw.op("act", lambda e: e.activation(out=xb[i], in_=xo[i], func=AF.Copy), reads=[bxo[i]], writes=[bxb[i]])
                    pb, pbuf = bk[t % 2]; pv = pb.bitcast(BF16)
                    for kc in range(KC):
                        fw.op("pe", lambda e, kc=kc: e.transpose(out=pv[:, kc * P:(kc + 1) * P], in_=xb[i][:, kc * P:(kc + 1) * P], identity=c["ident_bf"]), reads=[bxb[i], cb], writes=[pbuf])
                    T = xT[blk % 2]
                    fw.op("act", lambda e: e.activation(out=T[:, :, j * P:(j + 1) * P], in_=pv.rearrange("p (k t) -> p k t", k=KC), func=AF.Copy), reads=[pbuf], writes=[bT[blk % 2]])
                    if j == 3:
                        fw.dma("sp", f"f_sT{blk % 2}", lambda e: e.dma_start(out=self.XT[blk], in_=T), reads=[bT[blk % 2]], writes=[self.DB("XT")])
            fw.barrier()

    def load_params(self, st0, l):
        fw = self.fw; c = self.c
        if "pcol" not in c:
            c["pcol"] = self.sb(st0, "pcol_sb", [P, NCOL], F32); c["bpcol"] = Buf("pcol")
            c["prow_bf"] = self.sb(st0, "prow_bf", [1, 1032], BF16); c["prow_f"] = self.sb(st0, "prow_f", [1, NE], F32); c["bprow"] = Buf("prow")
            for nm, val in [("eps", EPS), ("one", 1.0), ("nln16", -math.log(16.0))]:
                c[nm] = self.sb(st0, "k_" + nm, [P, 1], F32)
                fw.op("pool", lambda e, nm=nm, val=val: e.memset(c[nm], val), writes=[c["b"]])
        fw.barrier()
        fw.dma("sp", "pcol", lambda e: e.dma_start(out=c["pcol"], in_=self.pcol[l]), writes=[c["bpcol"]])
        fw.dma("pool", "prow", lambda e: e.dma_start(out=c["prow_bf"], in_=self.prow[l, 0:1, 0:1032]), writes=[c["bprow"]])
        fw.dma("sp", "prowf", lambda e: e.dma_start(out=c["prow_f"], in_=self.prow[l, 0:1, PROW["r_b"]:PROW["r_b"] + NE]), writes=[c["bprow"]])

    def build(self):
        fw = self.fw
        with ExitStack() as st0:
            self.consts(st0)
            self.gathers(0, ("win", "sm"))
            self.zero_scratch()
            self.pass_x0(self.x_in, "x_in")
            for l in range(self.Lp):
                self.load_params(st0, l)
                x_src, xkey = (self.x_in, "x_in") if l == 0 else (self.XN[(l - 1) % 2], f"XN{(l - 1) % 2}")
                last = (l == self.Lp - 1)
                dst, dkey = (self.out, "out") if last else (self.XN[l % 2], f"XN{l % 2}")
                self.pass_c(l)
                self.pass_m1(l)
                self.pass_m2(l, x_src, xkey)
                self.pass_e(l)
                self.pass_f(l, dst, dkey, make_xt=not last)
            fw.barrier()
            self.finish()
        return self.nc

    def finish(self):
        fw = self.fw; nc = self.nc
        for k in ("cc", "gcp0", "gcp1", "gcp2", "gcp3"):
            if k in fw.dsem:
                for e in fw.engs:
                    fw._wait(e, k, fw.dsem[k][1])
        fin = nc.alloc_semaphore(name="fin")
        for e in ("pe", "act", "dve", "sp"):
            fw.engs[e].sem_inc(fin, 1)
        g = nc.gpsimd
        g.wait_ge(fin, 4)
        for k in fw.sem:
            g.sem_clear(fw.sem[k])
        for k, (sh, c) in fw.dsem.items():
            g.sem_clear(sh)
        g.sem_clear(fin)


def _pcol_host(b_in, dw_w, dw_b, cn_g, cn_b, co_b, qk_w, qk_b, mn_g, b1):
    a = np.zeros((P, NCOL), np.float32)
    def cols(v, n):
        return np.ascontiguousarray(v.reshape(n, P).T)
    a[:, PCOL["b_a"]:PCOL["b_a"] + 8] = cols(b_in[0:1024], 8)
    a[:, PCOL["b_g"]:PCOL["b_g"] + 8] = cols(b_in[1024:2048], 8)
    a[:, PCOL["b_q"]:PCOL["b_q"] + 8] = cols(b_in[OFF_Q:OFF_Q + 1024], 8)
    a[:, PCOL["b_k"]:PCOL["b_k"] + 8] = cols(b_in[OFF_K:OFF_K + 1024], 8)
    a[:, PCOL["b_o"]:PCOL["b_o"] + 8] = cols(b_in[OFF_O:OFF_O + 1024], 8)
    a[:, PCOL["b_ga"]:PCOL["b_ga"] + 8] = cols(b_in[OFF_GA:OFF_GA + 1024], 8)
    a[:, PCOL["b_gb"]:PCOL["b_gb"] + 8] = cols(b_in[OFF_GB:OFF_GB + 1024], 8)
    a[:, PCOL["dw_w"]:PCOL["dw_w"] + 8 * TAPS] = dw_w.reshape(TAPS, 8, P).transpose(2, 1, 0).reshape(P, 8 * TAPS)
    a[:, PCOL["dw_b"]:PCOL["dw_b"] + 8] = cols(dw_b, 8)
    a[:, PCOL["cn_g"]:PCOL["cn_g"] + 8] = cols(cn_g, 8)
    a[:, PCOL["cn_b"]:PCOL["cn_b"] + 8] = cols(cn_b, 8)
    a[:, PCOL["co_b"]:PCOL["co_b"] + 8] = cols(co_b, 8)
    a[:, PCOL["qk_w"]:PCOL["qk_w"] + 64] = qk_w.reshape(4, 16, P).transpose(2, 1, 0).reshape(P, 64)
    a[:, PCOL["qk_b"]:PCOL["qk_b"] + 16] = cols(qk_b, 16)
    a[:, PCOL["mn_g"]:PCOL["mn_g"] + 8] = cols(mn_g, 8)
    a[:, PCOL["b1"]:PCOL["b1"] + NE * 16] = b1.reshape(NE, 16, P).transpose(2, 0, 1).reshape(P, NE * 16)
    return a


def _prow_host(b_in, r_b, b2):
    a = np.zeros((1, NROW), np.float32)
    a[0, 0:1024] = b_in[OFF_V:OFF_V + 1024]
    a[0, 1024:1032] = b_in[OFF_I:OFF_I + 8]
    a[0, 1032:1064] = r_b
    a[0, 1064:] = b2.reshape(-1)
    return a


_NC_CACHE = {}


def _layer_inputs(ls, inp, core):
    W = lambda k: np.asarray(inp[k])
    sl = slice(core * P, (core + 1) * P)
    es = slice(core * 4, core * 4 + 4)
    d = {}
    d["w_in_sh"] = np.stack([W("w_in")[l][sl] for l in ls])
    d["co_sh"] = np.stack([W("conv_out_w")[l][sl] for l in ls])
    d["mo_sh"] = np.stack([W("mlstm_out_w")[l][sl] for l in ls])
    d["wo_sh"] = np.stack([W("w_out")[l][sl] for l in ls])
    d["w1_sh"] = np.stack([W("moe_w1")[l][es].reshape(4 * D, 2 * DFF) for l in ls])
    d["w2_sh"] = np.stack([W("moe_w2")[l][es].reshape(4 * DFF, D) for l in ls])
    d["pcol"] = np.stack([_pcol_host(W("b_in")[l], W("conv_dw_w")[l], W("conv_dw_b")[l], W("conv_norm_g")[l], W("conv_norm_b")[l], W("conv_out_b")[l],
                                     W("qk_conv_w")[l], W("qk_conv_b")[l], W("mlstm_norm_g")[l], W("moe_b1")[l]) for l in ls])
    d["prow"] = np.stack([_prow_host(W("b_in")[l], W("router_b")[l], W("moe_b2")[l]) for l in ls])
    d["pbc"] = np.stack([np.stack([W("ln1_g")[l], W("ln1_b")[l], W("ln2_g")[l], W("ln2_b")[l]]) for l in ls])
    d["rw"] = np.stack([W("router_w")[l] for l in ls])
    return d


def run_layers(x_full, inp, ls, debug=False):
    key = (len(ls), debug)
    nc = Builder(len(ls), debug=debug).build()
    xs = np.ascontiguousarray(x_full, dtype=np.float32).reshape(NCORES, NT, D)
    in_maps = []
    for c in range(NCORES):
        d = _layer_inputs(ls, inp, c)
        d["x"] = xs[c]
        in_maps.append(d)
    res = run_bass_kernel_spmd(nc, in_maps, core_ids=list(range(NCORES)))
    out = np.stack([r["out"] for r in res.results]).reshape(x_full.shape)
    return out, res


FUSED = True


def kernel(**inputs):
    x = np.asarray(inputs["x"], dtype=np.float32)
    if FUSED:
        out, _ = run_layers(x, inputs, list(range(DEPTH)))
        return out
    for l in range(DEPTH):
        x, _ = run_layers(x, inputs, [l])
    return x
```
